# Optimizing a Trainium2 kernel written in Bass

```python
import jax, jax.numpy as jnp
from jax import lax
import numpy as np

D_MODEL = 1024
BATCH = 8
SEQ = 4096
DEPTH = 1

GRID_W = 64
ROPE_THETA = 10000.0
Q_BLOCK = 128
EPS = 1e-6

MLA_HEADS = 8
MLA_NOPE = 64
MLA_ROPE = 32
MLA_QK = MLA_NOPE + MLA_ROPE
MLA_V = 64
Q_LORA = 768
KV_LORA = 256

GQA_HEADS = 8
GQA_KV_HEADS = 2
GQA_HEAD_DIM = 64

MLA_WIDTH = MLA_HEADS * MLA_V
GQA_WIDTH = GQA_HEADS * GQA_HEAD_DIM

SPLIT_SIZES = (
    Q_LORA,
    KV_LORA,
    MLA_ROPE,
    GQA_HEADS * GQA_HEAD_DIM,
    GQA_KV_HEADS * GQA_HEAD_DIM,
    GQA_KV_HEADS * GQA_HEAD_DIM,
    D_MODEL,
    D_MODEL,
)
IN_WIDTH = sum(SPLIT_SIZES)
SPLIT_POINTS = tuple(int(v) for v in np.cumsum(SPLIT_SIZES)[:-1])

N_EXPERTS = 16
CAPACITY_FACTOR = 2
EXPERT_FF = 1024

kernel_name = "hybrid_mla_gqa_axial_ec_moe_encoder"


def rmsnorm(x, g):
    xf = x.astype(jnp.float32)
    y = xf * lax.rsqrt(jnp.mean(xf * xf, axis=-1, keepdims=True) + EPS)
    return (y * g.astype(jnp.float32)).astype(x.dtype)


def axial_angles(n, rot_dim):
    rows = n // GRID_W
    row = jnp.broadcast_to(jnp.arange(rows)[:, None], (rows, GRID_W)).reshape(n).astype(jnp.float32)
    col = jnp.broadcast_to(jnp.arange(GRID_W)[None, :], (rows, GRID_W)).reshape(n).astype(jnp.float32)
    nf = rot_dim // 4
    inv = ROPE_THETA ** (-jnp.arange(nf, dtype=jnp.float32) / nf)
    ang = jnp.concatenate([row[:, None] * inv, col[:, None] * inv], axis=-1)
    return jnp.cos(ang), jnp.sin(ang)


def apply_rope(x, cos, sin):
    r = x.shape[-1]
    xf = x.astype(jnp.float32).reshape(*x.shape[:-1], r // 2, 2)
    x1, x2 = xf[..., 0], xf[..., 1]
    c = cos[None, :, None, :]
    s = sin[None, :, None, :]
    out = jnp.stack([x1 * c - x2 * s, x1 * s + x2 * c], axis=-1)
    return out.reshape(x.shape).astype(x.dtype)


def blocked_attention(q, k, v):
    b, s, h, d = q.shape
    kvh = k.shape[2]
    g = h // kvh
    dv = v.shape[-1]
    nb = s // Q_BLOCK
    scale = d ** -0.5
    qb = q.reshape(b, nb, Q_BLOCK, kvh, g, d).transpose(1, 0, 2, 3, 4, 5)

    def one_block(qblk):
        sc = jnp.einsum('bqkgd,bskd->bkgqs', qblk, k, preferred_element_type=jnp.float32) * scale
        p = jax.nn.softmax(sc, axis=-1).astype(v.dtype)
        return jnp.einsum('bkgqs,bskd->bqkgd', p, v)

    out = lax.map(one_block, qb)
    return out.transpose(1, 0, 2, 3, 4, 5).reshape(b, s, h, dv)


def ec_moe_sequence(h, w_router, b_router, w_exp_gate, w_exp_up, w_exp_down):
    s, d = h.shape
    cap = CAPACITY_FACTOR * s // N_EXPERTS
    logits = jnp.einsum('sd,de->se', h, w_router, preferred_element_type=jnp.float32) + b_router.astype(jnp.float32)
    aff = jax.nn.softmax(logits, axis=-1)
    gate, idx = lax.top_k(aff.T, cap)
    xe = h[idx]
    a = jnp.einsum('ecd,edf->ecf', xe, w_exp_gate)
    u = jnp.einsum('ecd,edf->ecf', xe, w_exp_up)
    y = jnp.einsum('ecf,efd->ecd', jax.nn.silu(a) * u, w_exp_down)
    y = y * gate[..., None].astype(y.dtype)
    return jnp.zeros((s, d), h.dtype).at[idx.reshape(-1)].add(y.reshape(-1, d))


def setup_inputs(seed: int = 0) -> dict:
    key = jax.random.key(seed)
    ks = jax.random.split(key, 24)
    f32 = jnp.float32

    def w(k, shape, fan_in):
        return jax.random.normal(k, shape, f32) * (fan_in ** -0.5)

    def gain(k, n):
        return 1.0 + 0.05 * jax.random.normal(k, (n,), f32)

    return {
        "x": jax.random.normal(ks[0], (BATCH, SEQ, D_MODEL), f32),
        "g_attn_norm": gain(ks[1], D_MODEL),
        "w_in": w(ks[2], (D_MODEL, IN_WIDTH), D_MODEL),
        "b_gate": 0.02 * jax.random.normal(ks[3], (2 * D_MODEL,), f32),
        "g_q_lat": gain(ks[4], Q_LORA),
        "w_q_up": w(ks[5], (Q_LORA, MLA_HEADS * MLA_QK), Q_LORA),
        "g_kv_lat": gain(ks[6], KV_LORA),
        "w_kv_up": w(ks[7], (KV_LORA, MLA_HEADS * (MLA_NOPE + MLA_V)), KV_LORA),
        "g_mla_qnorm": gain(ks[8], MLA_QK),
        "g_mla_knorm": gain(ks[9], MLA_QK),
        "g_gqa_qnorm": gain(ks[10], GQA_HEAD_DIM),
        "g_gqa_knorm": gain(ks[11], GQA_HEAD_DIM),
        "w_mla_branch": w(ks[12], (MLA_WIDTH, D_MODEL), MLA_WIDTH),
        "w_gqa_branch": w(ks[13], (GQA_WIDTH, D_MODEL), GQA_WIDTH),
        "w_out": w(ks[14], (D_MODEL, D_MODEL), D_MODEL),
        "g_ffn_norm": gain(ks[15], D_MODEL),
        "w_router": w(ks[16], (D_MODEL, N_EXPERTS), D_MODEL),
        "b_router": 0.01 * jax.random.normal(ks[17], (N_EXPERTS,), f32),
        "w_exp_gate": w(ks[18], (N_EXPERTS, D_MODEL, EXPERT_FF), D_MODEL),
        "w_exp_up": w(ks[19], (N_EXPERTS, D_MODEL, EXPERT_FF), D_MODEL),
        "w_exp_down": w(ks[20], (N_EXPERTS, EXPERT_FF, D_MODEL), EXPERT_FF),
    }


def reference(x, g_attn_norm, w_in, b_gate, g_q_lat, w_q_up, g_kv_lat, w_kv_up,
              g_mla_qnorm, g_mla_knorm, g_gqa_qnorm, g_gqa_knorm,
              w_mla_branch, w_gqa_branch, w_out, g_ffn_norm,
              w_router, b_router, w_exp_gate, w_exp_up, w_exp_down):
    b, s, _ = x.shape
    cos_mla, sin_mla = axial_angles(s, MLA_ROPE)
    cos_gqa, sin_gqa = axial_angles(s, GQA_HEAD_DIM)

    for _ in range(DEPTH):
        h = rmsnorm(x, g_attn_norm)
        proj = jnp.einsum('bsd,dn->bsn', h, w_in)
        q_lat, kv_lat, k_rope, q_g, k_g, v_g, gate_a, gate_b = jnp.split(proj, SPLIT_POINTS, axis=-1)

        c_q = rmsnorm(q_lat, g_q_lat)
        q_a = jnp.einsum('bsl,ln->bsn', c_q, w_q_up).reshape(b, s, MLA_HEADS, MLA_QK)
        c_kv = rmsnorm(kv_lat, g_kv_lat)
        kv_a = jnp.einsum('bsl,ln->bsn', c_kv, w_kv_up).reshape(b, s, MLA_HEADS, MLA_NOPE + MLA_V)
        k_nope, v_a = kv_a[..., :MLA_NOPE], kv_a[..., MLA_NOPE:]
        k_r = jnp.broadcast_to(k_rope[:, :, None, :], (b, s, MLA_HEADS, MLA_ROPE))
        k_a = jnp.concatenate([k_nope, k_r], axis=-1)
        q_a = rmsnorm(q_a, g_mla_qnorm)
        k_a = rmsnorm(k_a, g_mla_knorm)
        q_a = jnp.concatenate([q_a[..., :MLA_NOPE], apply_rope(q_a[..., MLA_NOPE:], cos_mla, sin_mla)], axis=-1)
        k_a = jnp.concatenate([k_a[..., :MLA_NOPE], apply_rope(k_a[..., MLA_NOPE:], cos_mla, sin_mla)], axis=-1)
        y_a = blocked_attention(q_a, k_a, v_a).reshape(b, s, MLA_WIDTH)

        q_b = rmsnorm(q_g.reshape(b, s, GQA_HEADS, GQA_HEAD_DIM), g_gqa_qnorm)
        k_b = rmsnorm(k_g.reshape(b, s, GQA_KV_HEADS, GQA_HEAD_DIM), g_gqa_knorm)
        v_b = v_g.reshape(b, s, GQA_KV_HEADS, GQA_HEAD_DIM)
        q_b = apply_rope(q_b, cos_gqa, sin_gqa)
        k_b = apply_rope(k_b, cos_gqa, sin_gqa)
        y_b = blocked_attention(q_b, k_b, v_b).reshape(b, s, GQA_WIDTH)

        ga = jax.nn.sigmoid(gate_a + b_gate[:D_MODEL])
        gb = jax.nn.sigmoid(gate_b + b_gate[D_MODEL:])
        merged = (ga * jnp.einsum('bsm,md->bsd', y_a, w_mla_branch)
                  + gb * jnp.einsum('bsm,md->bsd', y_b, w_gqa_branch))
        x = x + jnp.einsum('bsd,de->bse', merged, w_out)

        h2 = rmsnorm(x, g_ffn_norm)
        moe = jax.vmap(ec_moe_sequence, in_axes=(0, None, None, None, None, None))(
            h2, w_router, b_router, w_exp_gate, w_exp_up, w_exp_down)
        x = x + moe
    return x
```

```python
import contextlib
import numpy as np
import concourse.bass as bass
import concourse.mybir as mybir
from concourse.bass_utils import run_bass_kernel_spmd

F32 = mybir.dt.float32
BF16 = mybir.dt.bfloat16
I32 = mybir.dt.int32
AF = mybir.ActivationFunctionType
ALU = mybir.AluOpType
AX = mybir.AxisListType

D = 1024
S = 4096
NT = S // 128
EPS = 1e-6
NE = 16
CAP = 512
FF = 1024


class Buf:
    def __init__(self, name=""):
        self.name = name
        self.w = {}
        self.r = {}
        self.dsem = None
        self.dcount = 0
        self.excl = False


class Tile(Buf):
    def __init__(self, name, handle):
        super().__init__(name)
        self.t = handle

    def __getitem__(self, k):
        return self.t[k]


class Group:
    def __init__(self, sem):
        self.sem = sem
        self.count = 0


class Eng:
    def __init__(self, name, eng, sem):
        self.name = name
        self.eng = eng
        self.sem = sem
        self.count = 0
        self.known = {}


def _tv(tok):
    sem, val, grp = tok
    return val if grp is None else 16 * grp.count


class KB:
    def __init__(self, nc, es):
        self.nc = nc
        self.es = es
        self.E = {}
        for name, eng in (("pe", nc.tensor), ("act", nc.scalar), ("dve", nc.vector),
                          ("pool", nc.gpsimd), ("sp", nc.sync)):
            self.E[name] = Eng(name, eng, es.enter_context(nc.semaphore("sem_" + name)))
        self.dsems = {}
        self.nsem = 5
        self.arena_bytes = 204 * 1024
        base = (nc.sbuf_base + 63) // 64 * 64
        self.arena = es.enter_context(nc.sbuf_tensor("arena", [128, self.arena_bytes + 64], mybir.dt.uint8))
        self.arena_base = base
        self.ptr = 0
        self.hw = 0

    def sem(self, name):
        self.nsem += 1
        return self.es.enter_context(self.nc.semaphore(name))

    def group(self, name):
        g = Group(self.sem("g_" + name))
        self.dsems[("g", id(g))] = (g.sem, lambda g=g: 16 * g.count)
        return g

    def sb(self, name, shape, dt, es=None):
        esz = {F32: 4, BF16: 2, I32: 4}[dt]
        n = esz
        for d in shape[1:]:
            n *= d
        n = (n + 63) // 64 * 64
        assert self.ptr + n <= self.arena_bytes, ("SBUF arena overflow", name, self.ptr, n)
        h = self.nc.alloc_sbuf_tensor_at(name, list(shape), dt, offset=self.arena_base + self.ptr)
        self.ptr += n
        self.hw = max(self.hw, self.ptr)
        return Tile(name, h)

    def _collect(self, reads, writes, en=None):
        toks = {}

        def add(d, skip=None):
            for k, v in d.items():
                if k == skip:
                    continue
                if k not in toks or _tv(v) > _tv(toks[k]):
                    toks[k] = v

        for b in reads:
            add(b.w)
            if b.excl:
                add(b.r, skip=en)
        for b in writes:
            add(b.w)
            add(b.r)
        return toks

    def _wait(self, E, toks):
        for k, tok in toks.items():
            v = _tv(tok)
            if k == "pe" and E.name == "pe":
                continue
            if E.known.get(k, 0) >= v:
                continue
            E.eng.wait_ge(tok[0], v)
            E.known[k] = v

    def op(self, en, fn, reads=(), writes=(), inc=True):
        E = self.E[en]
        self._wait(E, self._collect(reads, writes, en))
        ins = fn(E.eng)
        n = E.count + 1
        if inc:
            ins.then_inc(E.sem, 1)
            E.count = n
        tok = (E.sem, n, None)
        for b in reads:
            b.r[en] = tok
        for b in writes:
            b.w = {en: tok}
            b.r = {}
        return ins

    def dma(self, q, out=None, in_=None, reads=(), writes=(), sb=None, grp=None, fn=None, merge=()):
        E = self.E[q]
        toks = self._collect(reads, writes)
        for b in merge:
            for k, v in b.r.items():
                if k not in toks or _tv(v) > _tv(toks[k]):
                    toks[k] = v
        if grp is None:
            b = sb
            if b.dsem is None:
                b.dsem = self.sem("d_" + b.name)
                self.dsems[("d", id(b))] = (b.dsem, lambda b=b: 16 * b.dcount)
            key = ("d", id(b))
            if b.dcount:
                toks[key] = (b.dsem, 16 * b.dcount, None)
            b.dcount += 1
            tok = (b.dsem, 16 * b.dcount, None)
            sem = b.dsem
        else:
            key = ("g", id(grp))
            toks.pop(key, None)
            grp.count += 1
            tok = (grp.sem, None, grp)
            sem = grp.sem
        self._wait(E, toks)
        ins = fn(E.eng) if fn else E.eng.dma_start(out=out, in_=in_)
        ins.then_inc(sem, 16)
        for b in reads:
            b.r[key] = tok
        for b in writes:
            b.w = {key: tok}
            b.r = {}
        for b in merge:
            b.w[key] = tok
        return ins

    def barrier(self):
        for E in self.E.values():
            for E2 in self.E.values():
                if E2 is E or E2.count == 0:
                    continue
                if E.known.get(E2.name, 0) < E2.count:
                    E.eng.wait_ge(E2.sem, E2.count)
                    E.known[E2.name] = E2.count
            for key, (sem, cur) in self.dsems.items():
                v = cur()
                if v and E.known.get(key, 0) < v:
                    E.eng.wait_ge(sem, v)
                    E.known[key] = v

    def finish(self):
        self.barrier()


def _axial(n, rot_dim):
    rows = n // 64
    row = np.repeat(np.arange(rows), 64).astype(np.float32)
    col = np.tile(np.arange(64), rows).astype(np.float32)
    nf = rot_dim // 4
    inv = (np.float32(10000.0) ** (-np.arange(nf, dtype=np.float32) / np.float32(nf))).astype(np.float32)
    ang = np.concatenate([row[:, None] * inv, col[:, None] * inv], axis=-1).astype(np.float32)
    return np.cos(ang).astype(np.float32), np.sin(ang).astype(np.float32)


def _tok_major(a):
    f = a.shape[1]
    return np.ascontiguousarray(a.reshape(NT, 128, f).transpose(1, 0, 2).reshape(128, NT * f))


C_ID = 0
C_IOTA = 128
C_TRI = 640
C_ONES = 768
C_P = 896
C_T = 897
C_COSM = 929
C_SINM = C_COSM + 512
C_COSG = C_SINM + 512
C_SING = C_COSG + 1024
NCONST = C_SING + 1024

V_GA = 0
V_GQ = 8
V_GKV = 14
V_BG = 16
V_GMQ = 32
V_GMK = 128
V_GGQ = 224
V_GGK = 288
V_BR = 352
V_GF = 368
NVEC = V_GF + 1024
NGAR = 1024


def make_consts():
    c = np.zeros((128, NCONST), np.float32)
    c[:, C_ID:C_ID + 128] = np.eye(128, dtype=np.float32)
    c[:, C_IOTA:C_IOTA + 512] = np.arange(512, dtype=np.float32)[None, :]
    c[:, C_TRI:C_TRI + 128] = np.triu(np.ones((128, 128), np.float32), 1)
    c[:, C_ONES:C_ONES + 128] = 1.0
    c[:, C_P] = np.arange(128, dtype=np.float32)
    c[:, C_T:C_T + 32] = np.arange(32, dtype=np.float32)[None, :]
    cm, sm = _axial(S, 32)
    cg, sg = _axial(S, 64)
    c[:, C_COSM:C_COSM + 512] = _tok_major(cm)
    c[:, C_SINM:C_SINM + 512] = _tok_major(sm)
    c[:, C_COSG:C_COSG + 1024] = _tok_major(cg)
    c[:, C_SING:C_SING + 1024] = _tok_major(sg)
    return c


def make_vecs(inp):
    v = np.zeros((128, NVEC), np.float32)
    f = lambda a, n: np.ascontiguousarray(np.asarray(a, np.float32).reshape(n, 128).T)
    rep = lambda a: np.broadcast_to(np.asarray(a, np.float32)[None, :], (128, a.shape[0]))
    v[:, V_GA:V_GA + 8] = f(inp["g_attn_norm"], 8)
    v[:, V_GQ:V_GQ + 6] = f(inp["g_q_lat"], 6)
    v[:, V_GKV:V_GKV + 2] = f(inp["g_kv_lat"], 2)
    v[:, V_BG:V_BG + 16] = f(inp["b_gate"], 16)
    v[:, V_GMQ:V_GMQ + 96] = rep(inp["g_mla_qnorm"])
    v[:, V_GMK:V_GMK + 96] = rep(inp["g_mla_knorm"])
    v[:, V_GGQ:V_GGQ + 64] = rep(inp["g_gqa_qnorm"])
    v[:, V_GGK:V_GGK + 64] = rep(inp["g_gqa_knorm"])
    v[:, V_BR:V_BR + 16] = rep(inp["b_router"])
    v[:, V_GF:V_GF + 1024] = rep(inp["g_ffn_norm"])
    return v


FILL_N = 0


def build(nt=NT, phases="ABDE", debug=False):
    nc = bass.Bass("TRN2", target_bir_lowering=False)
    s_len = nt * 128
    nst = nt // 4

    def din(name, shape, dt=F32):
        return nc.dram_tensor(name, list(shape), dt, kind="ExternalInput").ap()

    def dscr(name, shape, dt, out=False):
        return nc.dram_tensor(name, list(shape), dt, kind="ExternalOutput" if out else "Internal").ap()

    x_d = din("x", [S, D])
    w_in_d = din("w_in", [D, 3872])
    w_qup_d = din("w_q_up", [768, 768])
    w_kvup_d = din("w_kv_up", [256, 1024])
    w_mb_d = din("w_mla_branch", [512, D])
    w_gb_d = din("w_gqa_branch", [512, D])
    w_out_d = din("w_out", [D, D])
    w_r_d = din("w_router", [D, NE])
    if "E" in phases:
        w_eg_d = din("w_exp_gate", [NE, D, FF])
        w_eu_d = din("w_exp_up", [NE, D, FF])
        w_ed_d = din("w_exp_down", [NE, FF, D])
    vec_d = din("vecs", [128, NVEC])
    gar_d = din("gar", [128, NGAR])
    con_d = din("consts", [128, NCONST])
    out_d = nc.dram_tensor("out", [S, D], F32, kind="ExternalOutput").ap()

    dbg = debug
    qtm_d = dscr("qtm_s", [8, 96, S], BF16, dbg)
    ktm_d = dscr("ktm_s", [8, 96, S], BF16, dbg)
    qtg_d = dscr("qtg_s", [4, 128, S], BF16, dbg)
    ktg_d = dscr("ktg_s", [128, S], BF16, dbg)
    xnt_d = dscr("xnt_s", [NT, 128, D], BF16, dbg)
    h2_d = dscr("h2_s", [S, D], BF16, dbg)

    if "E" in phases:
        web_d = nc.dram_tensor("web_s", [3, NE, D, FF], BF16, kind="Internal").ap()
        web_b = [[Buf("web_b%d_%d" % (k, e_)) for e_ in range(NE)] for k in range(3)]
    es = contextlib.ExitStack()
    with es:
        kb = KB(nc, es)
        op, dma = kb.op, kb.dma

        con = kb.sb("con", [128, NCONST], F32)
        vec = kb.sb("vec", [128, NVEC], F32)
        ident_bf = kb.sb("ident_bf", [128, 128], BF16)
        ones_bf = kb.sb("ones_bf", [128, 128], BF16)
        tri_bf = kb.sb("tri_bf", [128, 128], BF16)
        gmq_s = kb.sb("gmq_s", [128, 96], F32)
        ggq_s = kb.sb("ggq_s", [128, 64], F32)
        ones_f = kb.sb("ones_f", [128, 128], F32)
        lg = kb.sb("lg", [128, NT, NE], F32)
        wr = kb.sb("wr", [128, 8, NE], F32)
        P0E = kb.ptr
        gar = kb.sb("gar", [128, D], F32)
        P0 = kb.ptr
        vm = kb.sb("vm", [128, NT, 8, 66], BF16)
        vg = kb.sb("vg", [128, NT, 2, 66], BF16)
        class Half(Buf):
            def __init__(self, name, handle, half):
                Buf.__init__(self, name)
                self.t = handle
                self.half = half
                self.excl = True

            def __getitem__(self, k):
                if not isinstance(k, tuple):
                    k = (k, slice(None))
                pk, ck = k
                a = 0 if ck.start is None else ck.start
                b = 512 if ck.stop is None else ck.stop
                return self.t[pk, self.half * 512 + a:self.half * 512 + b]

        banks = []
        pairs = []
        for i in range(4):
            h = es.enter_context(nc.psum_tensor("pbank%d" % i, [128, 1024], F32))
            pairs.append(h)
            banks.append(Half("bank%d" % (2 * i), h, 0))
            banks.append(Half("bank%d" % (2 * i + 1), h, 1))

        def bf(bank):
            return bank[:].bitcast(BF16)

        cast_jobs = []
        if "E" in phases:
            for e_ in range(NE):
                for k, src in enumerate((w_eg_d, w_eu_d, w_ed_d)):
                    cast_jobs.append((k, e_, src))
        cast_sems = [Buf("castsem%d" % i) for i in range(4)]

        def issue_casts(n):
            for _ in range(n):
                if not cast_jobs:
                    return
                k, e_, src = cast_jobs.pop(0)
                cs = cast_sems[0]
                dma("pool", out=web_d[k, e_].rearrange("(c p) n -> p c n", p=128),
                    in_=src[e_].rearrange("(c p) n -> p c n", p=128), writes=[web_b[k][e_]], sb=cs)

        g0 = kb.group("setup")
        dma("sp", out=con[:], in_=con_d, writes=[con], grp=g0)
        dma("sp", out=vec[:], in_=vec_d, writes=[vec], grp=g0)
        dma("sp", out=gar[:], in_=gar_d, writes=[gar], sb=gar)
        op("dve", lambda e: e.tensor_copy(out=ident_bf[:], in_=con[:, C_ID:C_ID + 128]), [con], [ident_bf])
        op("dve", lambda e: e.tensor_copy(out=ones_bf[:], in_=con[:, C_ONES:C_ONES + 128]), [con], [ones_bf])
        op("dve", lambda e: e.tensor_copy(out=tri_bf[:], in_=con[:, C_TRI:C_TRI + 128]), [con], [tri_bf])
        op("dve", lambda e: e.tensor_scalar(out=gmq_s[:], in0=vec[:, V_GMQ:V_GMQ + 96], scalar1=96.0 ** -0.5,
                                            scalar2=None, op0=ALU.mult), [vec], [gmq_s])
        op("dve", lambda e: e.tensor_scalar(out=ggq_s[:], in0=vec[:, V_GGQ:V_GGQ + 64], scalar1=64.0 ** -0.5,
                                            scalar2=None, op0=ALU.mult), [vec], [ggq_s])
        op("dve", lambda e: e.tensor_copy(out=ones_f[:], in_=con[:, C_ONES:C_ONES + 128]), [con], [ones_f])
        P1 = kb.ptr
        with nc.allow_non_contiguous_dma(reason="tiny router weight layout"):
            dma("sp", out=wr[:], in_=w_r_d.rearrange("(c p) e -> p c e", p=128), writes=[wr], sb=wr)
        op("pool", lambda e: e.memset(vm[:, :, :, 64:66], 1.0), [], [vm])
        op("pool", lambda e: e.memset(vg[:, :, :, 64:66], 1.0), [], [vg])

        qtm_b = [Buf("qtm_b%d" % i) for i in range(nst)]
        ktm_b = [Buf("ktm_b%d" % i) for i in range(nst)]
        qtg_b = [Buf("qtg_b%d" % i) for i in range(nst)]
        ktg_b = [Buf("ktg_b%d" % i) for i in range(nst)]
        xnt_b = [Buf("xnt_b%d" % i) for i in range(nt)]

        if "A" in phases:
            with contextlib.ExitStack() as ea:
                wA = kb.sb("wA", [128, 8, 1824], BF16, ea)
                wq = kb.sb("wq", [128, 6, 768], BF16, ea)
                wkv = kb.sb("wkv", [128, 2, 1024], BF16, ea)
                p_wst = kb.ptr
                wst = [kb.sb("wst%d" % i, [128, 1024], F32, ea) for i in range(2)]
                cols = [(0, 768, 0), (768, 1024, 768), (1056, 1568, 1024), (1568, 1696, 1536),
                        (1696, 1824, 1664), (1024, 1056, 1792)]
                gw = kb.group("wA")
                for (a_, b_, o) in cols:
                    dma("pool", out=wA[:, :, o:o + (b_ - a_)],
                        in_=w_in_d[:, a_:b_].rearrange("(c p) n -> p c n", p=128), writes=[wA], grp=gw)
                for c in range(6):
                    st = wst[c % 2]
                    dma("sp", out=st[:, 0:768], in_=w_qup_d[c * 128:(c + 1) * 128, :], writes=[st], sb=st)
                    op("dve", lambda e, st=st, c=c: e.tensor_scalar(
                        out=wq[:, c, :], in0=st[:, 0:768], scalar1=vec[:, V_GQ + c:V_GQ + c + 1], scalar2=None,
                        op0=ALU.mult), [st, vec], [wq])
                for c in range(2):
                    st = wst[c % 2]
                    dma("sp", out=st[:, 0:1024], in_=w_kvup_d[c * 128:(c + 1) * 128, :], writes=[st], sb=st)
                    op("dve", lambda e, st=st, c=c: e.tensor_scalar(
                        out=wkv[:, c, :], in0=st[:, 0:1024], scalar1=vec[:, V_GKV + c:V_GKV + c + 1], scalar2=None,
                        op0=ALU.mult), [st, vec], [wkv])

                kb.barrier()
                kb.ptr = p_wst
                xt = [kb.sb("xt%d" % i, [128, D], F32) for i in range(2)]
                junk2 = kb.sb("junk2", [128, 32], BF16)
                xn = kb.sb("xn", [128, D], BF16)
                xnT = [kb.sb("xnT%d" % i, [128, 8, 128], BF16) for i in range(2)]
                ms1 = kb.sb("ms1", [128, 4], F32)
                r1 = kb.sb("r1", [128, 4], F32)
                ms2 = kb.sb("ms2", [128, 4], F32)
                r2 = kb.sb("r2", [128, 4], F32)
                cqk = kb.sb("cqk", [128, 1024], BF16)
                cT = kb.sb("cT", [128, 8, 128], BF16)
                EQ = [kb.sb("EQ%d" % i, [128, 8, 96], F32) for i in range(2)]
                EKV = [kb.sb("EKV%d" % i, [128, 8, 128], F32) for i in range(2)]
                EG = [kb.sb("EG%d" % i, [128, 800], F32) for i in range(2)]
                msh = kb.sb("msh", [128, 32], F32)
                msr = kb.sb("msr", [128, 1], F32)
                rh = kb.sb("rh", [128, 32], F32)
                qn = kb.sb("qn", [128, 8, 96], F32)
                kn = kb.sb("kn", [128, 8, 96], F32)
                gn = kb.sb("gn", [128, 10, 64], F32)
                qg = kb.sb("qg", [128, 8, 96], F32)
                kg = kb.sb("kg", [128, 8, 96], F32)
                gg = kb.sb("gg", [128, 10, 64], F32)
                qf = kb.sb("qf", [128, 8, 96], BF16)
                kf = kb.sb("kf", [128, 8, 96], BF16)
                gf = kb.sb("gf", [128, 10, 64], BF16)
                rtq = [kb.sb("rtq%d" % i, [128, 8, 16], F32) for i in range(4)]
                rtk = [kb.sb("rtk%d" % i, [128, 8, 16], F32) for i in range(4)]
                rtg = [kb.sb("rtg%d" % i, [128, 10, 32], F32) for i in range(4)]
                QTs = kb.sb("QTs", [128, 8, 512], BF16)
                KTs = kb.sb("KTs", [128, 8, 512], BF16)
                QGs = kb.sb("QGs", [128, 4, 512], BF16)
                KGs = kb.sb("KGs", [128, 512], BF16)

                def rstd(ms, r, n):
                    op("act", lambda e: e.activation(out=r[:, 0:n], in_=ms[:, 0:n], func=AF.Ln, bias=EPS, scale=1.0),
                       [ms], [r])
                    op("act", lambda e: e.activation(out=r[:, 0:n], in_=r[:, 0:n], func=AF.Exp, scale=-0.5),
                       [r], [r])

                def rope(src, dst, h0, h1, off, npair, cos_ap, sin_ap, eng, rt):
                    nh = h1 - h0
                    sv = src[:, h0:h1, off:off + 2 * npair].rearrange("p h (i two) -> p h i two", two=2)
                    dv = dst[:, h0:h1, off:off + 2 * npair].rearrange("p h (i two) -> p h i two", two=2)
                    x1, x2 = sv[:, :, :, 0], sv[:, :, :, 1]
                    cb = cos_ap.unsqueeze(1).to_broadcast([128, nh, npair])
                    sbb = sin_ap.unsqueeze(1).to_broadcast([128, nh, npair])
                    t = [r_[:, 0:nh, 0:npair] for r_ in rt]
                    op(eng, lambda e: e.tensor_tensor(out=t[0], in0=x1, in1=cb, op=ALU.mult), [src, con], [rt[0]])
                    op(eng, lambda e: e.tensor_tensor(out=t[1], in0=x2, in1=sbb, op=ALU.mult), [src, con], [rt[1]])
                    op(eng, lambda e: e.tensor_tensor(out=t[2], in0=x1, in1=sbb, op=ALU.mult), [src, con], [rt[2]])
                    op(eng, lambda e: e.tensor_tensor(out=t[3], in0=x2, in1=cb, op=ALU.mult), [src, con], [rt[3]])
                    op(eng, lambda e: e.tensor_tensor(out=dv[:, :, :, 0], in0=t[0], in1=t[1], op=ALU.subtract),
                       [rt[0], rt[1]], [dst])
                    op(eng, lambda e: e.tensor_tensor(out=dv[:, :, :, 1], in0=t[2], in1=t[3], op=ALU.add),
                       [rt[2], rt[3]], [dst])

                B = banks

                def load_x(t):
                    X = xt[t % 2]
                    dma("sp", out=X[:], in_=x_d[t * 128:(t + 1) * 128, :], writes=[X], sb=X)

                def early(t):
                    par = t % 2
                    X = xt[par]
                    XT = xnT[par]
                    issue_casts(1)
                    if t + 1 < nt:
                        load_x(t + 1)
                    op("act", lambda e: e.activation(out=xn[:], in_=X[:], func=AF.Square, scale=float(D) ** -0.5,
                                                     accum_out=ms1[:, 0:1]), [X], [xn, ms1])
                    rstd(ms1, r1, 1)
                    op("dve", lambda e: e.scalar_tensor_tensor(out=xn[:], in0=X[:], scalar=r1[:, 0:1], in1=gar[:],
                                                               op0=ALU.mult, op1=ALU.mult), [X, r1, gar], [xn])
                    yield
                    for c in range(8):
                        op("pe", lambda e, c=c: e.transpose(out=bf(B[0])[:, c * 128:(c + 1) * 128],
                                                            in_=xn[:, c * 128:(c + 1) * 128], identity=ident_bf[:]),
                           [xn, ident_bf], [B[0]], inc=(c == 7))
                    op("act", lambda e: e.copy(out=XT[:].rearrange("p c s -> p (c s)"), in_=bf(B[0])), [B[0]], [XT])
                    dma("sp", out=xnt_d[t], in_=XT[:].rearrange("p c s -> p (c s)"), reads=[XT], writes=[xnt_b[t]],
                        sb=XT)
                    yield
                    blocks = [(0, 512), (512, 1024), (1024, 1536), (1536, 1824)]
                    for bi, (a, b) in enumerate(blocks):
                        for c in range(8):
                            op("pe", lambda e, bi=bi, a=a, b=b, c=c: e.matmul(
                                B[1 + bi][:, 0:b - a], lhsT=XT[:, c, :], rhs=wA[:, c, a:b], start=(c == 0),
                                stop=(c == 7)), [XT, wA], [B[1 + bi]], inc=(c == 7))
                        if bi == 1:
                            yield
                    B0, B1, B2, B3 = B[1], B[2], B[3], B[4]
                    yield
                    op("act", lambda e: e.activation(out=xn[:, 0:512], in_=B0[:], func=AF.Square,
                                                     scale=768.0 ** -0.5, accum_out=ms2[:, 0:1]), [B0], [xn, ms2])
                    op("act", lambda e: e.activation(out=xn[:, 512:768], in_=B1[:, 0:256], func=AF.Square,
                                                     scale=768.0 ** -0.5, accum_out=ms2[:, 1:2]), [B1], [xn, ms2])
                    op("act", lambda e: e.activation(out=xn[:, 768:1024], in_=B1[:, 256:512], func=AF.Square,
                                                     scale=256.0 ** -0.5, accum_out=ms2[:, 2:3]), [B1], [xn, ms2])
                    op("dve", lambda e: e.tensor_tensor(out=ms2[:, 0:1], in0=ms2[:, 0:1], in1=ms2[:, 1:2],
                                                        op=ALU.add), [ms2], [ms2])
                    rstd(ms2, r2, 3)
                    op("dve", lambda e: e.tensor_copy(out=EG[par][:, 0:512], in_=B2[:, :]), [B2], [EG[par]])
                    op("dve", lambda e: e.tensor_copy(out=EG[par][:, 512:800], in_=B3[:, 0:288]), [B3], [EG[par]])
                    yield
                    op("dve", lambda e: e.tensor_copy(out=cqk[:, 0:512], in_=B0[:]), [B0], [cqk])
                    op("dve", lambda e: e.tensor_copy(out=cqk[:, 512:1024], in_=B1[:, :]), [B1], [cqk])
                    yield
                    for c in range(8):
                        op("pe", lambda e, c=c: e.transpose(out=bf(B[0])[:, c * 128:(c + 1) * 128],
                                                            in_=cqk[:, c * 128:(c + 1) * 128], identity=ident_bf[:]),
                           [cqk, ident_bf], [B[0]], inc=(c == 7))
                    op("act", lambda e: e.copy(out=cT[:].rearrange("p c s -> p (c s)"), in_=bf(B[0])), [B[0]], [cT])
                    yield
                    PQ0, PQ1, PKV0, PKV1 = B[5], B[6], B[7], B[1]
                    for c in range(6):
                        op("pe", lambda e, c=c: e.matmul(PQ0[:, 0:480], lhsT=cT[:, c, :], rhs=wq[:, c, 0:480],
                                                         start=(c == 0), stop=(c == 5)), [cT, wq], [PQ0], inc=(c == 5))
                    for c in range(6):
                        op("pe", lambda e, c=c: e.matmul(PQ1[:, 0:288], lhsT=cT[:, c, :], rhs=wq[:, c, 480:768],
                                                         start=(c == 0), stop=(c == 5)), [cT, wq], [PQ1], inc=(c == 5))
                    for c in range(2):
                        op("pe", lambda e, c=c: e.matmul(PKV0[:], lhsT=cT[:, 6 + c, :], rhs=wkv[:, c, 0:512],
                                                         start=(c == 0), stop=(c == 1)), [cT, wkv], [PKV0], inc=(c == 1))
                    for c in range(2):
                        op("pe", lambda e, c=c: e.matmul(PKV1[:], lhsT=cT[:, 6 + c, :], rhs=wkv[:, c, 512:1024],
                                                         start=(c == 0), stop=(c == 1)), [cT, wkv], [PKV1], inc=(c == 1))
                    yield
                    eqf = EQ[par][:].rearrange("p h d -> p (h d)")
                    ekf = EKV[par][:].rearrange("p h d -> p (h d)")
                    op("act", lambda e: e.activation(out=eqf[:, 0:480], in_=PQ0[:, 0:480], func=AF.Copy,
                                                     scale=r2[:, 0:1]), [PQ0, r2], [EQ[par]])
                    op("dve", lambda e: e.tensor_scalar(out=eqf[:, 480:768], in0=PQ1[:, 0:288], scalar1=r2[:, 0:1],
                                                        scalar2=None, op0=ALU.mult), [PQ1, r2], [EQ[par]])
                    op("act", lambda e: e.activation(out=ekf[:, 0:512], in_=PKV0[:, :], func=AF.Copy,
                                                     scale=r2[:, 2:3]), [PKV0, r2], [EKV[par]])
                    op("dve", lambda e: e.tensor_scalar(out=ekf[:, 512:1024], in0=PKV1[:, :], scalar1=r2[:, 2:3],
                                                        scalar2=None, op0=ALU.mult), [PKV1, r2], [EKV[par]])
                    yield

                def late(t):
                    par = t % 2
                    j = t % 4
                    st_i = t // 4
                    Eq, Ekv, Eg = EQ[par], EKV[par], EG[par]
                    egq = Eg[:, 0:512].rearrange("p (h d) -> p h d", d=64)
                    egk = Eg[:, 512:640].rearrange("p (h d) -> p h d", d=64)
                    egv = Eg[:, 640:768].rearrange("p (h d) -> p h d", d=64)
                    ekr = Eg[:, 768:800]
                    op("act", lambda e: e.activation(out=qg[:], in_=Eq[:], func=AF.Square, scale=96.0 ** -0.5), [Eq], [qg])
                    op("act", lambda e: e.activation(out=kg[:, :, 0:64], in_=Ekv[:, :, 0:64], func=AF.Square,
                                                     scale=96.0 ** -0.5), [Ekv], [kg])
                    op("act", lambda e: e.activation(out=junk2[:, 0:32], in_=ekr, func=AF.Square,
                                                     scale=96.0 ** -0.5, accum_out=msr[:, 0:1]), [Eg], [junk2, msr])
                    op("act", lambda e: e.activation(out=gg[:].rearrange("p h d -> p (h d)"), in_=Eg[:, 0:640],
                                                     func=AF.Square, scale=64.0 ** -0.5), [Eg], [gg])
                    yield
                    op("dve", lambda e: e.tensor_reduce(out=msh[:, 0:8], in_=qg[:], axis=AX.X, op=ALU.add), [qg], [msh])
                    op("dve", lambda e: e.tensor_reduce(out=msh[:, 8:16], in_=kg[:, :, 0:64], axis=AX.X, op=ALU.add),
                       [kg], [msh])
                    op("dve", lambda e: e.tensor_scalar(out=msh[:, 8:16], in0=msh[:, 8:16], scalar1=msr[:, 0:1],
                                                        scalar2=None, op0=ALU.add), [msh, msr], [msh])
                    op("dve", lambda e: e.tensor_reduce(out=msh[:, 16:26], in_=gg[:], axis=AX.X, op=ALU.add), [gg], [msh])
                    rstd(msh, rh, 26)
                    yield
                    op("dve", lambda e: e.tensor_tensor(out=qn[:], in0=Eq[:],
                                                        in1=rh[:, 0:8].unsqueeze(2).to_broadcast([128, 8, 96]),
                                                        op=ALU.mult), [Eq, rh], [qn])
                    op("pool", lambda e: e.tensor_tensor(out=kn[:, :, 0:64], in0=Ekv[:, :, 0:64],
                                                         in1=rh[:, 8:16].unsqueeze(2).to_broadcast([128, 8, 64]),
                                                         op=ALU.mult), [Ekv, rh], [kn])
                    op("pool", lambda e: e.tensor_tensor(out=kn[:, :, 64:96],
                                                         in0=ekr.unsqueeze(1).to_broadcast([128, 8, 32]),
                                                         in1=rh[:, 8:16].unsqueeze(2).to_broadcast([128, 8, 32]),
                                                         op=ALU.mult), [Eg, rh], [kn])
                    op("dve", lambda e: e.tensor_tensor(out=gn[:, 0:8, :], in0=egq,
                                                        in1=rh[:, 16:24].unsqueeze(2).to_broadcast([128, 8, 64]),
                                                        op=ALU.mult), [Eg, rh], [gn])
                    op("dve", lambda e: e.tensor_tensor(out=gn[:, 8:10, :], in0=egk,
                                                        in1=rh[:, 24:26].unsqueeze(2).to_broadcast([128, 2, 64]),
                                                        op=ALU.mult), [Eg, rh], [gn])
                    op("act", lambda e: e.copy(out=vm[:, t, :, 0:64], in_=Ekv[:, :, 64:128]), [Ekv], [vm])
                    op("act", lambda e: e.copy(out=vg[:, t, :, 0:64], in_=egv), [Eg], [vg])
                    yield
                    op("pool", lambda e: e.tensor_tensor(out=qg[:], in0=qn[:],
                                                         in1=gmq_s[:].unsqueeze(1).to_broadcast([128, 8, 96]),
                                                         op=ALU.mult), [qn, gmq_s], [qg])
                    op("dve", lambda e: e.tensor_tensor(out=kg[:], in0=kn[:],
                                                        in1=vec[:, V_GMK:V_GMK + 96].unsqueeze(1).to_broadcast([128, 8, 96]),
                                                        op=ALU.mult), [kn, vec], [kg])
                    op("pool", lambda e: e.tensor_tensor(out=gg[:, 0:8, :], in0=gn[:, 0:8, :],
                                                         in1=ggq_s[:].unsqueeze(1).to_broadcast([128, 8, 64]),
                                                         op=ALU.mult), [gn, ggq_s], [gg])
                    op("dve", lambda e: e.tensor_tensor(out=gg[:, 8:10, :], in0=gn[:, 8:10, :],
                                                        in1=vec[:, V_GGK:V_GGK + 64].unsqueeze(1).to_broadcast([128, 2, 64]),
                                                        op=ALU.mult), [gn, vec], [gg])
                    yield
                    op("act", lambda e: e.copy(out=qf[:, :, 0:64], in_=qg[:, :, 0:64]), [qg], [qf])
                    op("act", lambda e: e.copy(out=kf[:, :, 0:64], in_=kg[:, :, 0:64]), [kg], [kf])
                    cm = con[:, C_COSM + t * 16:C_COSM + (t + 1) * 16]
                    sm = con[:, C_SINM + t * 16:C_SINM + (t + 1) * 16]
                    cgm = con[:, C_COSG + t * 32:C_COSG + (t + 1) * 32]
                    sgm = con[:, C_SING + t * 32:C_SING + (t + 1) * 32]
                    rope(kg, kf, 0, 8, 64, 16, cm, sm, "dve", rtk)
                    rope(qg, qf, 0, 8, 64, 16, cm, sm, "pool", rtq)
                    yield
                    rope(gg, gf, 0, 10, 0, 32, cgm, sgm, "pool", rtg)
                    yield
                    TQ, TK, TG = B[2], B[3], B[4]
                    for h in range(8):
                        op("pe", lambda e, h=h: e.transpose(out=bf(TK)[0:96, h * 128:(h + 1) * 128], in_=kf[:, h, :],
                                                            identity=ident_bf[:]), [kf, ident_bf], [TK], inc=(h == 7))
                    for h in range(8):
                        op("pe", lambda e, h=h: e.transpose(out=bf(TQ)[0:96, h * 128:(h + 1) * 128], in_=qf[:, h, :],
                                                            identity=ident_bf[:]), [qf, ident_bf], [TQ], inc=(h == 7))
                    op("dve", lambda e: e.tensor_copy(out=KTs[0:96, :, j * 128:(j + 1) * 128],
                                                      in_=bf(TK)[0:96, :].rearrange("p (h s) -> p h s", s=128)),
                       [TK], [KTs])
                    op("act", lambda e: e.copy(out=QTs[0:96, :, j * 128:(j + 1) * 128],
                                               in_=bf(TQ)[0:96, :].rearrange("p (h s) -> p h s", s=128)), [TQ], [QTs])
                    yield
                    for i in range(5):
                        op("pe", lambda e, i=i: e.transpose(
                            out=bf(TG)[:, i * 128:(i + 1) * 128],
                            in_=gf[:, 2 * i:2 * i + 2, :].rearrange("p h d -> p (h d)"),
                            identity=ident_bf[:]), [gf, ident_bf], [TG], inc=(i == 4))
                    op("act", lambda e: e.copy(out=QGs[:, :, j * 128:(j + 1) * 128],
                                               in_=bf(TG)[:, 0:512].rearrange("p (h s) -> p h s", s=128)), [TG], [QGs])
                    op("dve", lambda e: e.tensor_copy(out=KGs[:, j * 128:(j + 1) * 128], in_=bf(TG)[:, 512:640]),
                       [TG], [KGs])
                    if j == 3:
                        c0 = st_i * 512
                        dma("sp", out=qtm_d[:, :, c0:c0 + 512].rearrange("h d s -> d h s"), in_=QTs[0:96, :, :],
                            reads=[QTs], writes=[qtm_b[st_i]], sb=QTs)
                        dma("sp", out=ktm_d[:, :, c0:c0 + 512].rearrange("h d s -> d h s"), in_=KTs[0:96, :, :],
                            reads=[KTs], writes=[ktm_b[st_i]], sb=KTs)
                        dma("sp", out=qtg_d[:, :, c0:c0 + 512].rearrange("h d s -> d h s"), in_=QGs[:],
                            reads=[QGs], writes=[qtg_b[st_i]], sb=QGs)
                        dma("sp", out=ktg_d[:, c0:c0 + 512], in_=KGs[:], reads=[KGs], writes=[ktg_b[st_i]], sb=KGs)
                    yield

                load_x(0)
                for tick in range(nt + 1):
                    ge = early(tick) if tick < nt else iter(())
                    gl = late(tick - 1) if tick >= 1 else iter(())
                    alive = True
                    while alive:
                        a = next(ge, "end")
                        b = next(gl, "end")
                        alive = not (a == "end" and b == "end")
                kb.barrier()


        kb.ptr = P1
        yTa = kb.sb("yTa", [128, 4, S], BF16)
        yTb = kb.sb("yTb", [128, 4, S], BF16)
        P2 = kb.ptr
        p_sv0 = kb.ptr
        kb.ptr = P0
        wG = kb.sb("wG", [128, 8, 2048], BF16)
        pX = kb.ptr
        kb.ptr = p_sv0
        wG_loaded = [False]

        def load_wG():
            if not wG_loaded[0] and "D" in phases:
                wG_loaded[0] = True
                dma("pool", out=wG[:], in_=w_in_d[:, 1824:3872].rearrange("(c p) n -> p c n", p=128),
                    writes=[wG, vm, vg], sb=wG)

        if "B" in phases:
            Qb = [kb.sb("Qb%d" % i, [128, S], BF16) for i in range(2)]
            Kb = [kb.sb("Kb%d" % i, [128, S], BF16) for i in range(2)]
            NPT = 4
            pT = [kb.sb("pT%d" % i, [128, 1024], BF16) for i in range(NPT)]
            rl = kb.sb("rl", [128, 512], F32)
            rlb = [kb.sb("rlb%d" % i, [128, 512], F32) for i in range(2)]
            ytmp = [kb.sb("ytmp%d" % i, [128, 512], BF16) for i in range(2)]
            Vp = [kb.sb("Vp%d" % i, [128, NT, 128], BF16) for i in range(2)]
            for i in range(2):
                op("pool", lambda e, i=i: e.memset(Vp[i][:], 0.0), [], [Vp[i]])
            rl_d = nc.dram_tensor("rl_s", [4, 512], F32, kind="Internal").ap()
            rl_db = [Buf("rl_db%d" % i) for i in range(4)]
            NSP = 3
            SP_ = [Tile("spair%d" % i, pairs[i]) for i in range(NSP)]
            for t_ in SP_:
                t_.excl = True
            OB_ = banks[6:8]
            nqc = s_len // 512
            jobs = [("m", h) for h in range(8)] + [("g", h) for h in range(8)]
            qtm_all, ktm_all, qtg_all, ktg_all = qtm_b, ktm_b, qtg_b, ktg_b
            ktg_v = ktg_d.rearrange("(j d) s -> j d s", d=64)

            def load_job(i):
                kind, h = jobs[i]
                sl = i % 2
                issue_casts(1)
                Vsrc = vm if kind == "m" else vg
                hvv = h if kind == "m" else h // 4
                op("pool", lambda e: e.tensor_copy(out=Vp[sl][:, 0:nt, 0:65], in_=Vsrc[:, 0:nt, hvv, 0:65]),
                   [Vsrc], [Vp[sl]])
                if i == len(jobs) - 1:
                    load_wG()
                if kind == "m":
                    dma("sp", out=Qb[sl][0:96, 0:s_len], in_=qtm_d[h, :, 0:s_len], reads=qtm_all, writes=[Qb[sl]],
                        sb=Qb[sl])
                    dma("sp", out=Kb[sl][0:96, 0:s_len], in_=ktm_d[h, :, 0:s_len], reads=ktm_all, writes=[Kb[sl]],
                        sb=Kb[sl])
                else:
                    dma("sp", out=Qb[sl][:, 0:s_len], in_=qtg_d[h // 2, :, 0:s_len], reads=qtg_all, writes=[Qb[sl]],
                        sb=Qb[sl])
                    zh = 64 if h % 2 == 0 else 0
                    op("pool", lambda e: e.memset(Kb[sl][zh:zh + 64, 0:s_len], 0.0), [], [Kb[sl]])
                    dma("sp", out=Kb[sl][64 - zh:128 - zh, 0:s_len], in_=ktg_v[h // 4, :, 0:s_len], reads=ktg_all,
                        writes=[Kb[sl]], sb=Kb[sl])

            upairs = []
            for i, (kind, h) in enumerate(jobs):
                for qc in range(nqc):
                    for kp in range(nt // 2):
                        upairs.append((i, qc, kp))
            LAG = 2
            FIN_LAG = 4
            pending_fin = []
            load_job(0)
            yh_b = {}

            def emit_qk(p):
                i, qc, kp = upairs[p]
                kind, h = jobs[i]
                dk = 96 if kind == "m" else 128
                sl = i % 2
                Sp = SP_[p % NSP]
                for hh in range(2):
                    kt = 2 * kp + hh
                    op("pe", lambda e, kt=kt, hh=hh: e.matmul(
                        Sp[:, hh * 512:(hh + 1) * 512], lhsT=Kb[sl][0:dk, kt * 128:(kt + 1) * 128],
                        rhs=Qb[sl][0:dk, qc * 512:(qc + 1) * 512], start=True, stop=True),
                       [Kb[sl], Qb[sl]], [Sp], inc=(hh == 1))
                op("act", lambda e: e.activation(out=pT[p % NPT][:], in_=Sp[:, :], func=AF.Exp), [Sp], [pT[p % NPT]])

            def emit_pv(p, step):
                i, qc, kp = upairs[p]
                kind, h = jobs[i]
                V = Vp[i % 2]
                qi = i * nqc + qc
                O = OB_[qi % 2]
                for hh in range(2):
                    kt = 2 * kp + hh
                    op("pe", lambda e, kt=kt, hh=hh: e.matmul(
                        O[:, :], lhsT=V[:, kt, :], rhs=pT[p % NPT][:, hh * 512:(hh + 1) * 512],
                        start=(kt == 0), stop=(kt == nt - 1)), [V, pT[p % NPT]], [O], inc=(hh == 1))
                if kp == nt // 2 - 1:
                    yT = yTa if kind == "m" else yTb
                    key = (kind, h)
                    if key not in yh_b:
                        yh_b[key] = Buf("yh_%s%d" % key)
                    yb = yh_b[key]
                    rb = rlb[qi % 2]
                    op("dve", lambda e: e.reciprocal(out=rl[64:65, :], in_=O[64:65, :]), [O], [rl])
                    rsl = qi % 4
                    dma("pool", out=rl_d[rsl:rsl + 1, :], in_=rl[64:65, :], reads=[rl], writes=[rl_db[rsl]], sb=rl)
                    dma("pool", out=rb[0:64, :], in_=rl_d[rsl:rsl + 1, :].to_broadcast([64, 512]), reads=[rl_db[rsl]],
                        writes=[rb], sb=rb)

                    def fin():
                        if h % 2 == 0:
                            op("dve", lambda e: e.tensor_tensor(out=yT[0:64, h // 2, qc * 512:(qc + 1) * 512],
                                                                in0=O[0:64, :], in1=rb[0:64, :], op=ALU.mult),
                               [O, rb], [yb])
                        else:
                            yt = ytmp[(qi // 2) % 2]
                            op("dve", lambda e: e.tensor_tensor(out=yt[0:64, :], in0=O[0:64, :], in1=rb[0:64, :],
                                                                op=ALU.mult), [O, rb], [yt])
                            dma("pool", out=yT[64:128, h // 2, qc * 512:(qc + 1) * 512], in_=yt[0:64, :],
                                reads=[yt], writes=[yb], sb=yt)
                    pending_fin.append((step + FIN_LAG, fin))

            nU = len(upairs)
            for step in range(nU + LAG + FIN_LAG + 1):
                if step < nU:
                    emit_qk(step)
                if 0 <= step - LAG < nU:
                    emit_pv(step - LAG, step)
                if step < nU:
                    i_, qc_, kp_ = upairs[step]
                    if qc_ == 0 and kp_ == LAG and i_ + 1 < len(jobs):
                        load_job(i_ + 1)
                while pending_fin and pending_fin[0][0] <= step:
                    pending_fin.pop(0)[1]()
            issue_casts(100)
            kb.barrier()

        x1_b = [Buf("x1_b%d" % i) for i in range(nt)]
        h2_b = [Buf("h2_b%d" % i) for i in range(nt)]
        if "D" in phases:
            kb.ptr = pX
            h2fb = kb.sb("h2fb", [128, D], F32)
            h2T = kb.sb("h2T", [128, 8, 128], F32)
            assert kb.ptr <= P1
            kb.ptr = P2
            wmb = kb.sb("wmb", [128, 4, D], BF16)
            wgb = kb.sb("wgb", [128, 4, D], BF16)
            wout = kb.sb("wout", [128, 8, D], BF16)
            xnTc = kb.sb("xnTc", [128, 4, D], BF16)
            gsa = kb.sb("gsa", [128, 512], BF16)
            gsb = kb.sb("gsb", [128, 512], BF16)
            t1 = kb.sb("t1", [128, 512], F32)
            t2 = kb.sb("t2", [128, 512], F32)
            mT = kb.sb("mT", [128, 8, 512], BF16)
            xt2 = [kb.sb("xtD%d" % i, [128, D], F32) for i in range(2)]
            h2fa = kb.sb("h2f", [128, D], F32)
            h2fs = [h2fa, h2fb]
            h2bt = kb.sb("h2bt", [128, D], BF16)
            msD = kb.sb("msD", [128, 4], F32)
            rD = kb.sb("rD", [128, 4], F32)
            rDall = kb.sb("rDall", [128, NT], F32)
            load_wG()
            dma("pool", out=wmb[:], in_=w_mb_d.rearrange("(c p) n -> p c n", p=128), writes=[wmb], sb=wmb)
            dma("pool", out=wgb[:], in_=w_gb_d.rearrange("(c p) n -> p c n", p=128), writes=[wgb], sb=wgb)
            dma("pool", out=wout[:], in_=w_out_d.rearrange("(c p) n -> p c n", p=128), writes=[wout], sb=wout)
            GA, GB, ZA, ZB = banks[0], banks[1], banks[2], banks[3]
            X1 = banks[4:6]
            HT = banks[6:8]
            nsc = s_len // 512
            def load_xn(sc):
                dma("sp", out=xnTc[:], in_=xnt_d[4 * sc:4 * sc + 4].rearrange("t p n -> p t n"),
                    reads=xnt_b[4 * sc:4 * sc + 4], writes=[xnTc], sb=xnTc)

            def router_tr(t):
                h2f = h2fs[t % 2]
                for c in range(8):
                    op("pe", lambda e, c=c: e.transpose(out=HT[c // 4][:, (c % 4) * 128:(c % 4 + 1) * 128],
                                                        in_=h2f[:, c * 128:(c + 1) * 128],
                                                        identity=con[:, C_ID:C_ID + 128]),
                       [h2f, con], [HT[c // 4]], inc=(c % 4 == 3))
                for k2 in range(2):
                    op("act", lambda e, k2=k2: e.copy(out=h2T[:, 4 * k2:4 * k2 + 4, :].rearrange("p c s -> p (c s)"),
                                                      in_=HT[k2][:, :]), [HT[k2]], [h2T])

            def router_mm(t):
                for c in range(8):
                    op("pe", lambda e, c=c: e.matmul(RL[:, 0:NE], lhsT=h2T[:, c, :], rhs=wr[:, c, :],
                                                     start=(c == 0), stop=(c == 7)), [h2T, wr], [RL], inc=(c == 7))
                op("dve", lambda e: e.scalar_tensor_tensor(out=lg[:, t, :], in0=RL[:, 0:NE], scalar=rDall[:, t:t + 1],
                                                           in1=vec[:, V_BR:V_BR + NE], op0=ALU.mult, op1=ALU.add),
                   [RL, rDall, vec], [lg])

            RL = banks[0]
            pend_tr, pend_mm = [], []
            dma("sp", out=xt2[0][:], in_=x_d[0:128, :], writes=[xt2[0]], sb=xt2[0])

            def slot():
                if pend_mm:
                    router_mm(pend_mm.pop(0))
                if pend_tr:
                    t_ = pend_tr.pop(0)
                    router_tr(t_)
                    pend_mm.append(t_)
            load_xn(0)
            for sc in range(nsc):
                for dc in range(8):
                    if dc == 0:
                        slot()
                    for (G, off) in ((GA, 0), (GB, 1024)):
                        for c in range(8):
                            op("pe", lambda e, G=G, off=off, c=c: e.matmul(
                                G[:, :], lhsT=wG[:, c, off + dc * 128:off + (dc + 1) * 128],
                                rhs=xnTc[:, :, c * 128:(c + 1) * 128], start=(c == 0), stop=(c == 7)),
                               [wG, xnTc], [G], inc=(c == 7))
                    for (Z, wb, yT) in ((ZA, wmb, yTa), (ZB, wgb, yTb)):
                        for i in range(4):
                            op("pe", lambda e, Z=Z, wb=wb, yT=yT, i=i: e.matmul(
                                Z[:, :], lhsT=wb[:, i, dc * 128:(dc + 1) * 128],
                                rhs=yT[:, i, sc * 512:(sc + 1) * 512], start=(i == 0), stop=(i == 3)),
                               [wb, yT], [Z], inc=(i == 3))
                    op("act", lambda e: e.activation(out=gsa[:], in_=GA[:, :], func=AF.Sigmoid,
                                                     bias=vec[:, V_BG + dc:V_BG + dc + 1], scale=1.0), [GA, vec], [gsa])
                    op("act", lambda e: e.activation(out=gsb[:], in_=GB[:, :], func=AF.Sigmoid,
                                                     bias=vec[:, V_BG + 8 + dc:V_BG + 8 + dc + 1], scale=1.0),
                       [GB, vec], [gsb])
                    op("dve", lambda e: e.tensor_tensor(out=t1[:], in0=ZA[:, :], in1=gsa[:], op=ALU.mult), [ZA, gsa], [t1])
                    op("dve", lambda e: e.tensor_tensor(out=t2[:], in0=ZB[:, :], in1=gsb[:], op=ALU.mult), [ZB, gsb], [t2])
                    op("dve", lambda e: e.tensor_tensor(out=mT[:, dc, :], in0=t1[:], in1=t2[:], op=ALU.add),
                       [t1, t2], [mT])
                if sc + 1 < nsc:
                    load_xn(sc + 1)
                for j in range(4):
                    t = 4 * sc + j
                    X = xt2[t % 2]
                    h2f = h2fs[t % 2]
                    if t + 1 < nt:
                        Xn = xt2[(t + 1) % 2]
                        dma("sp", out=Xn[:], in_=x_d[(t + 1) * 128:(t + 2) * 128, :], writes=[Xn], sb=Xn)
                    for hf in range(2):
                        for dc in range(8):
                            op("pe", lambda e, hf=hf, dc=dc: e.matmul(
                                X1[hf][:, :], lhsT=mT[:, dc, j * 128:(j + 1) * 128],
                                rhs=wout[:, dc, hf * 512:(hf + 1) * 512], start=(dc == 0), stop=(dc == 7)),
                               [mT, wout], [X1[hf]], inc=(dc == 7))
                    for hf in range(2):
                        op("dve", lambda e, hf=hf: e.tensor_tensor(out=X[:, hf * 512:(hf + 1) * 512], in0=X1[hf][:, :],
                                                                   in1=X[:, hf * 512:(hf + 1) * 512], op=ALU.add),
                           [X1[hf], X], [X])
                    slot()
                    dma("sp", out=out_d[t * 128:(t + 1) * 128, :], in_=X[:], reads=[X], writes=[x1_b[t]], sb=X)
                    op("act", lambda e: e.activation(out=h2bt[:], in_=X[:], func=AF.Square, scale=float(D) ** -0.5,
                                                     accum_out=msD[:, 0:1]), [X], [h2bt, msD])
                    op("act", lambda e: e.activation(out=rD[:, 0:1], in_=msD[:, 0:1], func=AF.Ln, bias=EPS, scale=1.0),
                       [msD], [rD])
                    op("act", lambda e: e.activation(out=rDall[:, t:t + 1], in_=rD[:, 0:1], func=AF.Exp, scale=-0.5),
                       [rD], [rDall])
                    op("dve", lambda e: e.tensor_tensor(out=h2f[:], in0=X[:], in1=vec[:, V_GF:V_GF + D], op=ALU.mult),
                       [X, vec], [h2f])
                    op("dve", lambda e: e.tensor_scalar(out=h2bt[:], in0=h2f[:], scalar1=rDall[:, t:t + 1],
                                                        scalar2=None, op0=ALU.mult), [h2f, rDall], [h2bt])
                    dma("sp", out=h2_d[t * 128:(t + 1) * 128, :], in_=h2bt[:], reads=[h2bt], writes=[h2_b[t]], sb=h2bt)
                    pend_tr.append(t)
            while pend_tr or pend_mm:
                slot()
            kb.barrier()

        if "E" in phases:
            kb.ptr = P0E
            cap = nt * 16
            ncc = cap // 128
            NTE = nt * NE
            aff = kb.sb("aff", [128, nt, NE], F32)
            p_dead = kb.ptr
            ex = kb.sb("ex", [128, NT, NE], F32)
            base = kb.sb("base", [128, NT, NE], F32)
            pos = kb.sb("pos", [128, NT, NE], F32)
            gr1 = kb.sb("gr1", [128, NT, NE], F32)
            assert kb.ptr - p_dead == 8192
            p_dead2 = kb.ptr
            cmpb = kb.sb("cmpb", [128, NE, NT], BF16)
            ghi = kb.sb("ghi", [128, NT, NE], BF16)
            maskb = kb.sb("maskb", [128, NT, NE], BF16)
            padb = kb.sb("padb", [128, 512], BF16)
            assert kb.ptr - p_dead2 == 4096
            mx = kb.sb("mx", [128, nt], F32)
            se = kb.sb("se", [128, nt], F32)
            rse = kb.sb("rse", [128, nt], F32)
            part = kb.sb("part", [128, NE], F32)
            lo = kb.sb("lo", [128, NE], F32)
            mid = kb.sb("mid", [128, NE], F32)
            sst = kb.sb("sst", [128, NE], F32)
            posm = kb.sb("posm", [128, nt, NE], F32)
            vals = kb.sb("vals", [128, nt, NE, 6], BF16)
            RB = banks[0]
            op("dve", lambda e: e.tensor_reduce(out=mx[:], in_=lg[:, 0:nt, :], axis=AX.X, op=ALU.max), [lg], [mx])
            op("dve", lambda e: e.tensor_tensor(out=ex[:, 0:nt, :], in0=lg[:, 0:nt, :],
                                                in1=mx[:].unsqueeze(2).to_broadcast([128, nt, NE]), op=ALU.subtract),
               [lg, mx], [ex])
            op("act", lambda e: e.activation(out=ex[:, 0:nt, :], in_=ex[:, 0:nt, :], func=AF.Exp), [ex], [ex])
            op("dve", lambda e: e.tensor_reduce(out=se[:], in_=ex[:, 0:nt, :], axis=AX.X, op=ALU.add), [ex], [se])
            op("dve", lambda e: e.reciprocal(out=rse[:], in_=se[:]), [se], [rse])
            op("dve", lambda e: e.tensor_tensor(out=aff[:], in0=ex[:, 0:nt, :],
                                                in1=rse[:].unsqueeze(2).to_broadcast([128, nt, NE]), op=ALU.mult),
               [ex, rse], [aff])
            affT = ex[:, 0:nt, :].rearrange("p t e -> p (t e)").rearrange("p (e t) -> p e t", t=nt)
            op("dve", lambda e: e.tensor_copy(out=affT, in_=aff[:].rearrange("p t e -> p e t")), [aff], [ex])
            op("dve", lambda e: e.memset(lo[:], 0.0), [], [lo])
            op("dve", lambda e: e.memset(mid[:], 0.5), [], [mid])
            NIT = 34
            for k in range(NIT):
                half = 2.0 ** -(k + 1)
                op("dve", lambda e: e.tensor_tensor(out=cmpb[:, :, 0:nt], in0=affT,
                                                    in1=mid[:].unsqueeze(2).to_broadcast([128, NE, nt]), op=ALU.is_ge),
                   [ex, mid], [cmpb])
                op("dve", lambda e: e.tensor_reduce(out=part[:], in_=cmpb[:, :, 0:nt], axis=AX.X, op=ALU.add), [cmpb], [part])
                op("pe", lambda e: e.matmul(RB[:, 0:NE], lhsT=ones_f[:], rhs=part[:], start=True, stop=True),
                   [ones_f, part], [RB])
                op("dve", lambda e, half=half: e.tensor_scalar(out=sst[:], in0=RB[:, 0:NE], scalar1=cap - 0.5,
                                                               scalar2=half, op0=ALU.is_ge, op1=ALU.mult), [RB], [sst])
                op("dve", lambda e: e.tensor_tensor(out=lo[:], in0=lo[:], in1=sst[:], op=ALU.add), [lo, sst], [lo])
                op("dve", lambda e, half=half: e.tensor_scalar(out=mid[:], in0=lo[:], scalar1=half * 0.5, scalar2=None,
                                                               op0=ALU.add), [lo], [mid])
            op("dve", lambda e: e.tensor_tensor(out=maskb[:, 0:nt, :], in0=aff[:],
                                                in1=lo[:].unsqueeze(1).to_broadcast([128, nt, NE]), op=ALU.is_ge),
               [aff, lo], [maskb])
            WB, TB = banks[1], banks[2]
            mflat = maskb[:, 0:nt, :].rearrange("p t e -> p (t e)")
            op("pe", lambda e: e.matmul(WB[:, 0:NTE], lhsT=tri_bf[:], rhs=mflat, start=True, stop=True),
               [tri_bf, maskb], [WB])
            op("pe", lambda e: e.matmul(TB[:, 0:NTE], lhsT=ones_bf[:], rhs=mflat, start=True, stop=True),
               [ones_bf, maskb], [TB])
            op("dve", lambda e: e.memset(base[:, 0, :], 0.0), [], [base])
            for t in range(1, nt):
                op("dve", lambda e, t=t: e.tensor_tensor(out=base[:, t, :], in0=base[:, t - 1, :],
                                                         in1=TB[:, (t - 1) * NE:t * NE], op=ALU.add), [base, TB], [base])
            op("dve", lambda e: e.tensor_tensor(out=pos[:, 0:nt, :].rearrange("p t e -> p (t e)"), in0=WB[:, 0:NTE],
                                                in1=base[:, 0:nt, :].rearrange("p t e -> p (t e)"), op=ALU.add), [WB, base], [pos])
            op("dve", lambda e: e.scalar_tensor_tensor(out=posm[:], in0=pos[:, 0:nt, :], scalar=1.0, in1=maskb[:, 0:nt, :],
                                                       op0=ALU.add, op1=ALU.mult), [pos, maskb], [posm])
            op("dve", lambda e: e.tensor_scalar(out=posm[:], in0=posm[:], scalar1=-1.0, scalar2=None, op0=ALU.add),
               [posm], [posm])
            op("dve", lambda e: e.memset(vals[:], 0.0), [], [vals])
            op("dve", lambda e: e.tensor_copy(out=vals[:, :, :, 0],
                                              in_=con[:, C_T:C_T + nt].unsqueeze(2).to_broadcast([128, nt, NE])),
               [con], [vals])
            op("dve", lambda e: e.tensor_copy(out=vals[:, :, :, 1],
                                              in_=con[:, C_P:C_P + 1].unsqueeze(2).to_broadcast([128, nt, NE])),
               [con], [vals])
            op("dve", lambda e: e.tensor_copy(out=ghi[:, 0:nt, :], in_=aff[:]), [aff], [ghi])
            op("dve", lambda e: e.tensor_copy(out=vals[:, :, :, 2], in_=ghi[:, 0:nt, :]), [ghi], [vals])
            op("dve", lambda e: e.tensor_tensor(out=gr1[:, 0:nt, :], in0=aff[:], in1=ghi[:, 0:nt, :], op=ALU.subtract), [aff, ghi], [gr1])
            op("dve", lambda e: e.tensor_copy(out=ghi[:, 0:nt, :], in_=gr1[:, 0:nt, :]), [gr1], [ghi])
            op("dve", lambda e: e.tensor_copy(out=vals[:, :, :, 3], in_=ghi[:, 0:nt, :]), [ghi], [vals])
            op("dve", lambda e: e.tensor_tensor(out=gr1[:, 0:nt, :], in0=gr1[:, 0:nt, :], in1=ghi[:, 0:nt, :], op=ALU.subtract), [gr1, ghi], [gr1])
            op("dve", lambda e: e.tensor_copy(out=vals[:, :, :, 4], in_=gr1[:, 0:nt, :]), [gr1], [vals])

            Wg = [kb.sb("Wg%d" % i, [128, 8, FF], BF16) for i in range(2)]
            Wu = [kb.sb("Wu%d" % i, [128, 8, FF], BF16) for i in range(2)]
            Wd = [kb.sb("Wd%d" % i, [128, 8, D], BF16) for i in range(2)]
            oh = [kb.sb("oh%d" % i, [128, 512], BF16) for i in range(4)]
            selsb = [kb.sb("selsb%d" % i, [128, 4, 8], F32) for i in range(2)]
            idx = [kb.sb("idx%d" % i, [128, 4], I32) for i in range(2)]
            gate = [kb.sb("gate%d" % i, [128, 4], F32) for i in range(2)]
            xe = [kb.sb("xe%d" % i, [128, 4, D], BF16) for i in range(2)]
            xeT = kb.sb("xeT", [128, 8, cap], BF16)
            hT = kb.sb("hT", [128, 8, cap], BF16)
            sg = [kb.sb("sg%d" % i, [128, cap], F32) for i in range(2)]
            yo = [kb.sb("yo%d" % i, [128, D], F32) for i in range(2)]
            p_save = kb.ptr
            kb.ptr = p_dead
            yo += [kb.sb("yo%d" % i, [128, D], F32) for i in (2, 3)]
            kb.ptr = p_dead2
            oh += [kb.sb("oh%d" % i, [128, 512], BF16) for i in (4, 5, 6, 7)]
            kb.ptr = p_save
            out_grp = [Buf("out_grp%d" % i) for i in range(2)]
            SEL = banks[0]
            SELB = banks[7]
            TP = banks[1:3]
            AB_, UB_ = banks[3], banks[4]
            YB = banks[5:7]

            def load_w(e_):
                sl = e_ % 2
                for k, W_ in enumerate((Wg, Wu, Wd)):
                    dma("sp", out=W_[sl][:], in_=web_d[k, e_].rearrange("(c p) n -> p c n", p=128),
                        reads=[web_b[k][e_]], writes=[W_[sl]], sb=W_[sl])

            ohc = [0]
            selT = kb.sb("selT", [128, cap], F32)

            def select_dve(e_):
                for t in range(nt):
                    o = oh[t % 8]
                    op("dve", lambda e, o=o, t=t: e.tensor_scalar(
                        out=o[:, 0:cap], in0=con[:, C_IOTA:C_IOTA + cap], scalar1=posm[:, t, e_:e_ + 1], scalar2=None,
                        op0=ALU.is_equal), [con, posm], [o])
                    yield

            def select(e_):
                sl = e_ % 2
                for t in range(nt):
                    o = oh[t % 8]
                    op("pe", lambda e, o=o, t=t: e.matmul(SEL[0:6, 0:cap], lhsT=vals[:, t, e_, :], rhs=o[:, 0:cap],
                                                          start=(t == 0), stop=(t == nt - 1)), [o, vals], [SEL],
                       inc=True)
                    yield
                op("act", lambda e: e.copy(out=selT[0:6, :], in_=SEL[0:6, 0:cap]), [SEL], [selT])
                for cc in range(ncc):
                    op("pe", lambda e, cc=cc: e.transpose(out=SELB[:, cc * 8:cc * 8 + 6],
                                                          in_=selT[0:6, cc * 128:(cc + 1) * 128],
                                                          identity=con[0:6, C_ID:C_ID + 6]), [selT, con], [SELB],
                       inc=(cc == ncc - 1))
                op("act", lambda e: e.copy(out=selsb[sl][:, 0:ncc, 0:6],
                                           in_=SELB[:, 0:ncc * 8].rearrange("p (c k) -> p c k", k=8)[:, :, 0:6]),
                   [SELB], [selsb[sl]])
                op("dve", lambda e: e.scalar_tensor_tensor(out=idx[sl][:, 0:ncc], in0=selsb[sl][:, 0:ncc, 0], scalar=128.0,
                                                           in1=selsb[sl][:, 0:ncc, 1], op0=ALU.mult, op1=ALU.add),
                   [selsb[sl]], [idx[sl]])
                op("dve", lambda e: e.tensor_reduce(out=gate[sl][:, 0:ncc], in_=selsb[sl][:, 0:ncc, 2:5], axis=AX.X,
                                                    op=ALU.add), [selsb[sl]], [gate[sl]])
                for cc in range(ncc):
                    dma("pool", reads=h2_b[0:nt] + [idx[sl]], writes=[xe[sl]], sb=xe[sl],
                        fn=lambda e, cc=cc: e.indirect_dma_start(
                            out=xe[sl][:, cc, :], out_offset=None, in_=h2_d[0:s_len, :],
                            in_offset=bass.IndirectOffsetOnAxis(ap=idx[sl][:, cc:cc + 1], axis=0)))

            def compute(e_, sel=None, seld=None):
                sl = e_ % 2

                def adv(g, n):
                    if g is not None:
                        for _ in range(n):
                            next(g, None)
                for cc in range(ncc):
                    T_ = TP[cc % 2]
                    for c in range(8):
                        op("pe", lambda e, c=c: e.transpose(out=bf(T_)[:, c * 128:(c + 1) * 128],
                                                            in_=xe[sl][:, cc, c * 128:(c + 1) * 128],
                                                            identity=ident_bf[:]), [xe[sl], ident_bf], [T_], inc=(c == 7))
                    eng = "act" if cc % 2 == 0 else "dve"
                    if eng == "act":
                        op("act", lambda e: e.copy(out=xeT[:, :, cc * 128:(cc + 1) * 128],
                                                   in_=bf(T_).rearrange("p (c s) -> p c s", s=128)), [T_], [xeT])
                    else:
                        op("dve", lambda e: e.tensor_copy(out=xeT[:, :, cc * 128:(cc + 1) * 128],
                                                          in_=bf(T_).rearrange("p (c s) -> p c s", s=128)), [T_], [xeT])
                for fc in range(8):
                    for c in range(8):
                        op("pe", lambda e, c=c: e.matmul(AB_[:, 0:cap], lhsT=Wg[sl][:, c, fc * 128:(fc + 1) * 128],
                                                         rhs=xeT[:, c, :], start=(c == 0), stop=(c == 7)),
                           [Wg[sl], xeT], [AB_], inc=(c == 7))
                    for c in range(8):
                        op("pe", lambda e, c=c: e.matmul(UB_[:, 0:cap], lhsT=Wu[sl][:, c, fc * 128:(fc + 1) * 128],
                                                         rhs=xeT[:, c, :], start=(c == 0), stop=(c == 7)),
                           [Wu[sl], xeT], [UB_], inc=(c == 7))
                    s_ = sg[fc % 2]
                    adv(seld, (nt + 3) // 4)
                    op("act", lambda e: e.activation(out=s_[:], in_=AB_[:, 0:cap], func=AF.Silu), [AB_], [s_])
                    op("dve", lambda e: e.tensor_tensor(out=hT[:, fc, :], in0=UB_[:, 0:cap], in1=s_[:], op=ALU.mult),
                       [UB_, s_], [hT])
                    adv(sel, (nt + 3) // 4)
                    if fc == 3:
                        adv(seld, 1000)
                        adv(sel, 1000)
                adv(seld, 1000)
                adv(sel, 1000)
                out_grp[e_ % 2].w = {}
                out_grp[e_ % 2].r = {}
                for cc in range(ncc):
                    y_ = yo[cc % 4]
                    for hf in range(2):
                        Y = YB[hf]
                        for fc in range(8):
                            op("pe", lambda e, fc=fc: e.matmul(Y[:, :], lhsT=hT[:, fc, cc * 128:(cc + 1) * 128],
                                                               rhs=Wd[sl][:, fc, hf * 512:(hf + 1) * 512],
                                                               start=(fc == 0), stop=(fc == 7)),
                               [hT, Wd[sl]], [Y], inc=(fc == 7))
                        if hf == 0:
                            op("act", lambda e: e.activation(out=y_[:, 0:512], in_=Y[:, :], func=AF.Copy,
                                                             scale=gate[sl][:, cc:cc + 1]), [Y, gate[sl]], [y_])
                        else:
                            op("dve", lambda e: e.tensor_scalar(out=y_[:, 512:1024], in0=Y[:, :],
                                                                scalar1=gate[sl][:, cc:cc + 1], scalar2=None,
                                                                op0=ALU.mult), [Y, gate[sl]], [y_])
                    dma("pool", reads=[y_, idx[sl], out_grp[(e_ + 1) % 2]], merge=[out_grp[e_ % 2]], sb=y_,
                        fn=lambda e, cc=cc, y_=y_: e.indirect_dma_start(
                            out=out_d[0:s_len, :], out_offset=bass.IndirectOffsetOnAxis(ap=idx[sl][:, cc:cc + 1], axis=0),
                            in_=y_[:], in_offset=None, compute_op=ALU.add))

            n_exp = NE
            load_w(0)
            load_w(1)
            g0d, g0p = select_dve(0), select(0)
            for _ in range(nt):
                next(g0d, None)
                next(g0p, None)
            for _ in g0p:
                pass
            for e_ in range(n_exp):
                more = e_ + 1 < n_exp
                compute(e_, select(e_ + 1) if more else None, select_dve(e_ + 1) if more else None)
                if e_ + 2 < n_exp:
                    load_w(e_ + 2)
            kb.barrier()

        if debug and "D" not in phases:
            vm_o = nc.dram_tensor("vm_o", [128, NT * 8 * 66], BF16, kind="ExternalOutput").ap()
            vg_o = nc.dram_tensor("vg_o", [128, NT * 2 * 66], BF16, kind="ExternalOutput").ap()
            dma("sp", out=vm_o[:, 0:nt * 528], in_=vm[:, 0:nt].rearrange("p t h d -> p (t h d)"), reads=[vm], sb=vm)
            dma("sp", out=vg_o[:, 0:nt * 132], in_=vg[:, 0:nt].rearrange("p t h d -> p (t h d)"), reads=[vg], sb=vg)

        if debug and "B" in phases and "E" not in phases:
            ya_o = nc.dram_tensor("ya_o", [128, 4 * S], BF16, kind="ExternalOutput").ap()
            yb_o = nc.dram_tensor("yb_o", [128, 4 * S], BF16, kind="ExternalOutput").ap()
            dma("sp", out=ya_o.rearrange("p (i s) -> p i s", i=4)[:, :, 0:s_len], in_=yTa[:, :, 0:s_len], reads=[yTa], sb=yTa)
            dma("sp", out=yb_o.rearrange("p (i s) -> p i s", i=4)[:, :, 0:s_len], in_=yTb[:, :, 0:s_len], reads=[yTb], sb=yTb)
        if debug and "D" in phases:
            lg_o = nc.dram_tensor("lg_o", [128, NT * NE], F32, kind="ExternalOutput").ap()
            dma("sp", out=lg_o[:, 0:nt * NE], in_=lg[:, 0:nt, :].rearrange("p t e -> p (t e)"), reads=[lg], sb=lg)
        kb.finish()
        print("sbuf high water:", kb.hw, "semaphores used:", kb.nsem, {k: v.count for k, v in kb.E.items()})
    return nc


def make_in_maps(inputs):
    consts = make_consts()
    vecs = make_vecs(inputs)
    gar = np.ascontiguousarray(np.broadcast_to(np.asarray(inputs["g_attn_norm"], np.float32)[None, :], (128, D)))
    shared = {k: np.ascontiguousarray(np.asarray(inputs[k], np.float32)) for k in
              ("w_in", "w_q_up", "w_kv_up", "w_mla_branch", "w_gqa_branch", "w_out", "w_router",
               "w_exp_gate", "w_exp_up", "w_exp_down")}
    x = np.asarray(inputs["x"], np.float32)
    maps = []
    for b in range(x.shape[0]):
        m = dict(shared)
        m["x"] = np.ascontiguousarray(x[b])
        m["vecs"] = vecs
        m["gar"] = gar
        m["consts"] = consts
        maps.append(m)
    return maps


def kernel(**inputs):
    nc = build()
    maps = make_in_maps(inputs)
    res = run_bass_kernel_spmd(nc, maps, core_ids=list(range(8)))
    return np.stack([np.asarray(r["out"], np.float32) for r in res.results], axis=0)
```

```python
import contextlib
import numpy as np
import concourse.bass as bass
import concourse.mybir as mybir
from concourse.bass_utils import run_bass_kernel_spmd

F32 = mybir.dt.float32
BF16 = mybir.dt.bfloat16
I32 = mybir.dt.int32
AF = mybir.ActivationFunctionType
ALU = mybir.AluOpType
AX = mybir.AxisListType

D = 1024
S = 4096
NT = S // 128
EPS = 1e-6
NE = 16
CAP = 512
FF = 1024


class Buf:
    def __init__(self, name=""):
        self.name = name
        self.w = {}
        self.r = {}
        self.dsem = None
        self.dcount = 0
        self.excl = False


class Tile(Buf):
    def __init__(self, name, handle):
        super().__init__(name)
        self.t = handle

    def __getitem__(self, k):
        return self.t[k]


class Group:
    def __init__(self, sem):
        self.sem = sem
        self.count = 0


class Eng:
    def __init__(self, name, eng, sem):
        self.name = name
        self.eng = eng
        self.sem = sem
        self.count = 0
        self.known = {}


def _tv(tok):
    sem, val, grp = tok
    return val if grp is None else 16 * grp.count


class KB:
    def __init__(self, nc, es):
        self.nc = nc
        self.es = es
        self.E = {}
        for name, eng in (("pe", nc.tensor), ("act", nc.scalar), ("dve", nc.vector),
                          ("pool", nc.gpsimd), ("sp", nc.sync)):
            self.E[name] = Eng(name, eng, es.enter_context(nc.semaphore("sem_" + name)))
        self.dsems = {}
        self.nsem = 5
        self.arena_bytes = 204 * 1024
        base = (nc.sbuf_base + 63) // 64 * 64
        self.arena = es.enter_context(nc.sbuf_tensor("arena", [128, self.arena_bytes + 64], mybir.dt.uint8))
        self.arena_base = base
        self.ptr = 0
        self.hw = 0

    def sem(self, name):
        self.nsem += 1
        return self.es.enter_context(self.nc.semaphore(name))

    def group(self, name):
        g = Group(self.sem("g_" + name))
        self.dsems[("g", id(g))] = (g.sem, lambda g=g: 16 * g.count)
        return g

    def sb(self, name, shape, dt, es=None):
        esz = {F32: 4, BF16: 2, I32: 4}[dt]
        n = esz
        for d in shape[1:]:
            n *= d
        n = (n + 63) // 64 * 64
        assert self.ptr + n <= self.arena_bytes, ("SBUF arena overflow", name, self.ptr, n)
        h = self.nc.alloc_sbuf_tensor_at(name, list(shape), dt, offset=self.arena_base + self.ptr)
        self.ptr += n
        self.hw = max(self.hw, self.ptr)
        return Tile(name, h)

    def _collect(self, reads, writes, en=None):
        toks = {}

        def add(d, skip=None):
            for k, v in d.items():
                if k == skip:
                    continue
                if k not in toks or _tv(v) > _tv(toks[k]):
                    toks[k] = v

        for b in reads:
            add(b.w)
            if b.excl:
                add(b.r, skip=en)
        for b in writes:
            add(b.w)
            add(b.r)
        return toks

    def _wait(self, E, toks):
        for k, tok in toks.items():
            v = _tv(tok)
            if k == "pe" and E.name == "pe":
                continue
            if E.known.get(k, 0) >= v:
                continue
            E.eng.wait_ge(tok[0], v)
            E.known[k] = v

    def op(self, en, fn, reads=(), writes=(), inc=True):
        E = self.E[en]
        self._wait(E, self._collect(reads, writes, en))
        ins = fn(E.eng)
        n = E.count + 1
        if inc:
            ins.then_inc(E.sem, 1)
            E.count = n
        tok = (E.sem, n, None)
        for b in reads:
            b.r[en] = tok
        for b in writes:
            b.w = {en: tok}
            b.r = {}
        return ins

    def dma(self, q, out=None, in_=None, reads=(), writes=(), sb=None, grp=None, fn=None, merge=()):
        E = self.E[q]
        toks = self._collect(reads, writes)
        for b in merge:
            for k, v in b.r.items():
                if k not in toks or _tv(v) > _tv(toks[k]):
                    toks[k] = v
        if grp is None:
            b = sb
            if b.dsem is None:
                b.dsem = self.sem("d_" + b.name)
                self.dsems[("d", id(b))] = (b.dsem, lambda b=b: 16 * b.dcount)
            key = ("d", id(b))
            if b.dcount:
                toks[key] = (b.dsem, 16 * b.dcount, None)
            b.dcount += 1
            tok = (b.dsem, 16 * b.dcount, None)
            sem = b.dsem
        else:
            key = ("g", id(grp))
            toks.pop(key, None)
            grp.count += 1
            tok = (grp.sem, None, grp)
            sem = grp.sem
        self._wait(E, toks)
        ins = fn(E.eng) if fn else E.eng.dma_start(out=out, in_=in_)
        ins.then_inc(sem, 16)
        for b in reads:
            b.r[key] = tok
        for b in writes:
            b.w = {key: tok}
            b.r = {}
        for b in merge:
            b.w[key] = tok
        return ins

    def barrier(self):
        for E in self.E.values():
            for E2 in self.E.values():
                if E2 is E or E2.count == 0:
                    continue
                if E.known.get(E2.name, 0) < E2.count:
                    E.eng.wait_ge(E2.sem, E2.count)
                    E.known[E2.name] = E2.count
            for key, (sem, cur) in self.dsems.items():
                v = cur()
                if v and E.known.get(key, 0) < v:
                    E.eng.wait_ge(sem, v)
                    E.known[key] = v

    def finish(self):
        self.barrier()


def _axial(n, rot_dim):
    rows = n // 64
    row = np.repeat(np.arange(rows), 64).astype(np.float32)
    col = np.tile(np.arange(64), rows).astype(np.float32)
    nf = rot_dim // 4
    inv = (np.float32(10000.0) ** (-np.arange(nf, dtype=np.float32) / np.float32(nf))).astype(np.float32)
    ang = np.concatenate([row[:, None] * inv, col[:, None] * inv], axis=-1).astype(np.float32)
    return np.cos(ang).astype(np.float32), np.sin(ang).astype(np.float32)


def _tok_major(a):
    f = a.shape[1]
    return np.ascontiguousarray(a.reshape(NT, 128, f).transpose(1, 0, 2).reshape(128, NT * f))


C_ID = 0
C_IOTA = 128
C_TRI = 640
C_ONES = 768
C_P = 896
C_T = 897
C_COSM = 929
C_SINM = C_COSM + 512
C_COSG = C_SINM + 512
C_SING = C_COSG + 1024
NCONST = C_SING + 1024

V_GA = 0
V_GQ = 8
V_GKV = 14
V_BG = 16
V_GMQ = 32
V_GMK = 128
V_GGQ = 224
V_GGK = 288
V_BR = 352
V_GF = 368
NVEC = V_GF + 1024
NGAR = 1024


def make_consts():
    c = np.zeros((128, NCONST), np.float32)
    c[:, C_ID:C_ID + 128] = np.eye(128, dtype=np.float32)
    c[:, C_IOTA:C_IOTA + 512] = np.arange(512, dtype=np.float32)[None, :]
    c[:, C_TRI:C_TRI + 128] = np.triu(np.ones((128, 128), np.float32), 1)
    c[:, C_ONES:C_ONES + 128] = 1.0
    c[:, C_P] = np.arange(128, dtype=np.float32)
    c[:, C_T:C_T + 32] = np.arange(32, dtype=np.float32)[None, :]
    cm, sm = _axial(S, 32)
    cg, sg = _axial(S, 64)
    c[:, C_COSM:C_COSM + 512] = _tok_major(cm)
    c[:, C_SINM:C_SINM + 512] = _tok_major(sm)
    c[:, C_COSG:C_COSG + 1024] = _tok_major(cg)
    c[:, C_SING:C_SING + 1024] = _tok_major(sg)
    return c


def make_vecs(inp):
    v = np.zeros((128, NVEC), np.float32)
    f = lambda a, n: np.ascontiguousarray(np.asarray(a, np.float32).reshape(n, 128).T)
    rep = lambda a: np.broadcast_to(np.asarray(a, np.float32)[None, :], (128, a.shape[0]))
    v[:, V_GA:V_GA + 8] = f(inp["g_attn_norm"], 8)
    v[:, V_GQ:V_GQ + 6] = f(inp["g_q_lat"], 6)
    v[:, V_GKV:V_GKV + 2] = f(inp["g_kv_lat"], 2)
    v[:, V_BG:V_BG + 16] = f(inp["b_gate"], 16)
    v[:, V_GMQ:V_GMQ + 96] = rep(inp["g_mla_qnorm"])
    v[:, V_GMK:V_GMK + 96] = rep(inp["g_mla_knorm"])
    v[:, V_GGQ:V_GGQ + 64] = rep(inp["g_gqa_qnorm"])
    v[:, V_GGK:V_GGK + 64] = rep(inp["g_gqa_knorm"])
    v[:, V_BR:V_BR + 16] = rep(inp["b_router"])
    v[:, V_GF:V_GF + 1024] = rep(inp["g_ffn_norm"])
    return v


FILL_N = 0


def build(nt=NT, phases="ABDE", debug=False):
    nc = bass.Bass("TRN2", target_bir_lowering=False)
    s_len = nt * 128
    nst = nt // 4

    def din(name, shape, dt=F32):
        return nc.dram_tensor(name, list(shape), dt, kind="ExternalInput").ap()

    def dscr(name, shape, dt, out=False):
        return nc.dram_tensor(name, list(shape), dt, kind="ExternalOutput" if out else "Internal").ap()

    x_d = din("x", [S, D])
    w_in_d = din("w_in", [D, 3872])
    w_qup_d = din("w_q_up", [768, 768])
    w_kvup_d = din("w_kv_up", [256, 1024])
    w_mb_d = din("w_mla_branch", [512, D])
    w_gb_d = din("w_gqa_branch", [512, D])
    w_out_d = din("w_out", [D, D])
    w_r_d = din("w_router", [D, NE])
    if "E" in phases:
        w_eg_d = din("w_exp_gate", [NE, D, FF])
        w_eu_d = din("w_exp_up", [NE, D, FF])
        w_ed_d = din("w_exp_down", [NE, FF, D])
    vec_d = din("vecs", [128, NVEC])
    gar_d = din("gar", [128, NGAR])
    con_d = din("consts", [128, NCONST])
    out_d = nc.dram_tensor("out", [S, D], F32, kind="ExternalOutput").ap()

    dbg = debug
    qtm_d = dscr("qtm_s", [8, 96, S], BF16, dbg)
    ktm_d = dscr("ktm_s", [8, 96, S], BF16, dbg)
    qtg_d = dscr("qtg_s", [4, 128, S], BF16, dbg)
    ktg_d = dscr("ktg_s", [128, S], BF16, dbg)
    xnt_d = dscr("xnt_s", [NT, 128, D], BF16, dbg)
    h2_d = dscr("h2_s", [S, D], BF16, dbg)

    if "E" in phases:
        web_d = nc.dram_tensor("web_s", [3, NE, D, FF], BF16, kind="Internal").ap()
        web_b = [[Buf("web_b%d_%d" % (k, e_)) for e_ in range(NE)] for k in range(3)]
    es = contextlib.ExitStack()
    with es:
        kb = KB(nc, es)
        op, dma = kb.op, kb.dma

        con = kb.sb("con", [128, NCONST], F32)
        vec = kb.sb("vec", [128, NVEC], F32)
        ident_bf = kb.sb("ident_bf", [128, 128], BF16)
        ones_bf = kb.sb("ones_bf", [128, 128], BF16)
        tri_bf = kb.sb("tri_bf", [128, 128], BF16)
        gmq_s = kb.sb("gmq_s", [128, 96], F32)
        ggq_s = kb.sb("ggq_s", [128, 64], F32)
        ones_f = kb.sb("ones_f", [128, 128], F32)
        lg = kb.sb("lg", [128, NT, NE], F32)
        wr = kb.sb("wr", [128, 8, NE], F32)
        P0E = kb.ptr
        gar = kb.sb("gar", [128, D], F32)
        P0 = kb.ptr
        vm = kb.sb("vm", [128, NT, 8, 66], BF16)
        vg = kb.sb("vg", [128, NT, 2, 66], BF16)
        class Half(Buf):
            def __init__(self, name, handle, half):
                Buf.__init__(self, name)
                self.t = handle
                self.half = half
                self.excl = True

            def __getitem__(self, k):
                if not isinstance(k, tuple):
                    k = (k, slice(None))
                pk, ck = k
                a = 0 if ck.start is None else ck.start
                b = 512 if ck.stop is None else ck.stop
                return self.t[pk, self.half * 512 + a:self.half * 512 + b]

        banks = []
        pairs = []
        for i in range(4):
            h = es.enter_context(nc.psum_tensor("pbank%d" % i, [128, 1024], F32))
            pairs.append(h)
            banks.append(Half("bank%d" % (2 * i), h, 0))
            banks.append(Half("bank%d" % (2 * i + 1), h, 1))

        def bf(bank):
            return bank[:].bitcast(BF16)

        cast_jobs = []
        if "E" in phases:
            for e_ in range(NE):
                for k, src in enumerate((w_eg_d, w_eu_d, w_ed_d)):
                    cast_jobs.append((k, e_, src))
        cast_sems = [Buf("castsem%d" % i) for i in range(4)]

        def issue_casts(n):
            for _ in range(n):
                if not cast_jobs:
                    return
                k, e_, src = cast_jobs.pop(0)
                cs = cast_sems[0]
                dma("pool", out=web_d[k, e_].rearrange("(c p) n -> p c n", p=128),
                    in_=src[e_].rearrange("(c p) n -> p c n", p=128), writes=[web_b[k][e_]], sb=cs)

        g0 = kb.group("setup")
        dma("sp", out=con[:], in_=con_d, writes=[con], grp=g0)
        dma("sp", out=vec[:], in_=vec_d, writes=[vec], grp=g0)
        dma("sp", out=gar[:], in_=gar_d, writes=[gar], sb=gar)
        op("dve", lambda e: e.tensor_copy(out=ident_bf[:], in_=con[:, C_ID:C_ID + 128]), [con], [ident_bf])
        op("dve", lambda e: e.tensor_copy(out=ones_bf[:], in_=con[:, C_ONES:C_ONES + 128]), [con], [ones_bf])
        op("dve", lambda e: e.tensor_copy(out=tri_bf[:], in_=con[:, C_TRI:C_TRI + 128]), [con], [tri_bf])
        op("dve", lambda e: e.tensor_scalar(out=gmq_s[:], in0=vec[:, V_GMQ:V_GMQ + 96], scalar1=96.0 ** -0.5,
                                            scalar2=None, op0=ALU.mult), [vec], [gmq_s])
        op("dve", lambda e: e.tensor_scalar(out=ggq_s[:], in0=vec[:, V_GGQ:V_GGQ + 64], scalar1=64.0 ** -0.5,
                                            scalar2=None, op0=ALU.mult), [vec], [ggq_s])
        op("dve", lambda e: e.tensor_copy(out=ones_f[:], in_=con[:, C_ONES:C_ONES + 128]), [con], [ones_f])
        P1 = kb.ptr
        with nc.allow_non_contiguous_dma(reason="tiny router weight layout"):
            dma("sp", out=wr[:], in_=w_r_d.rearrange("(c p) e -> p c e", p=128), writes=[wr], sb=wr)
        op("pool", lambda e: e.memset(vm[:, :, :, 64:66], 1.0), [], [vm])
        op("pool", lambda e: e.memset(vg[:, :, :, 64:66], 1.0), [], [vg])

        qtm_b = [Buf("qtm_b%d" % i) for i in range(nst)]
        ktm_b = [Buf("ktm_b%d" % i) for i in range(nst)]
        qtg_b = [Buf("qtg_b%d" % i) for i in range(nst)]
        ktg_b = [Buf("ktg_b%d" % i) for i in range(nst)]
        xnt_b = [Buf("xnt_b%d" % i) for i in range(nt)]

        if "A" in phases:
            with contextlib.ExitStack() as ea:
                wA = kb.sb("wA", [128, 8, 1824], BF16, ea)
                wq = kb.sb("wq", [128, 6, 768], BF16, ea)
                wkv = kb.sb("wkv", [128, 2, 1024], BF16, ea)
                p_wst = kb.ptr
                wst = [kb.sb("wst%d" % i, [128, 1024], F32, ea) for i in range(2)]
                cols = [(0, 768, 0), (768, 1024, 768), (1056, 1568, 1024), (1568, 1696, 1536),
                        (1696, 1824, 1664), (1024, 1056, 1792)]
                gw = kb.group("wA")
                for (a_, b_, o) in cols:
                    dma("pool", out=wA[:, :, o:o + (b_ - a_)],
                        in_=w_in_d[:, a_:b_].rearrange("(c p) n -> p c n", p=128), writes=[wA], grp=gw)
                for c in range(6):
                    st = wst[c % 2]
                    dma("sp", out=st[:, 0:768], in_=w_qup_d[c * 128:(c + 1) * 128, :], writes=[st], sb=st)
                    op("dve", lambda e, st=st, c=c: e.tensor_scalar(
                        out=wq[:, c, :], in0=st[:, 0:768], scalar1=vec[:, V_GQ + c:V_GQ + c + 1], scalar2=None,
                        op0=ALU.mult), [st, vec], [wq])
                for c in range(2):
                    st = wst[c % 2]
                    dma("sp", out=st[:, 0:1024], in_=w_kvup_d[c * 128:(c + 1) * 128, :], writes=[st], sb=st)
                    op("dve", lambda e, st=st, c=c: e.tensor_scalar(
                        out=wkv[:, c, :], in0=st[:, 0:1024], scalar1=vec[:, V_GKV + c:V_GKV + c + 1], scalar2=None,
                        op0=ALU.mult), [st, vec], [wkv])

                kb.barrier()
                kb.ptr = p_wst
                xt = [kb.sb("xt%d" % i, [128, D], F32) for i in range(2)]
                junk2 = kb.sb("junk2", [128, 32], BF16)
                xn = kb.sb("xn", [128, D], BF16)
                xnT = [kb.sb("xnT%d" % i, [128, 8, 128], BF16) for i in range(2)]
                ms1 = kb.sb("ms1", [128, 4], F32)
                r1 = kb.sb("r1", [128, 4], F32)
                ms2 = kb.sb("ms2", [128, 4], F32)
                r2 = kb.sb("r2", [128, 4], F32)
                cqk = kb.sb("cqk", [128, 1024], BF16)
                cT = kb.sb("cT", [128, 8, 128], BF16)
                EQ = [kb.sb("EQ%d" % i, [128, 8, 96], F32) for i in range(2)]
                EKV = [kb.sb("EKV%d" % i, [128, 8, 128], F32) for i in range(2)]
                EG = [kb.sb("EG%d" % i, [128, 800], F32) for i in range(2)]
                msh = kb.sb("msh", [128, 32], F32)
                msr = kb.sb("msr", [128, 1], F32)
                rh = kb.sb("rh", [128, 32], F32)
                qn = kb.sb("qn", [128, 8, 96], F32)
                kn = kb.sb("kn", [128, 8, 96], F32)
                gn = kb.sb("gn", [128, 10, 64], F32)
                qg = kb.sb("qg", [128, 8, 96], F32)
                kg = kb.sb("kg", [128, 8, 96], F32)
                gg = kb.sb("gg", [128, 10, 64], F32)
                qf = kb.sb("qf", [128, 8, 96], BF16)
                kf = kb.sb("kf", [128, 8, 96], BF16)
                gf = kb.sb("gf", [128, 10, 64], BF16)
                rtq = [kb.sb("rtq%d" % i, [128, 8, 16], F32) for i in range(4)]
                rtk = [kb.sb("rtk%d" % i, [128, 8, 16], F32) for i in range(4)]
                rtg = [kb.sb("rtg%d" % i, [128, 10, 32], F32) for i in range(4)]
                QTs = kb.sb("QTs", [128, 8, 512], BF16)
                KTs = kb.sb("KTs", [128, 8, 512], BF16)
                QGs = kb.sb("QGs", [128, 4, 512], BF16)
                KGs = kb.sb("KGs", [128, 512], BF16)

                def rstd(ms, r, n):
                    op("act", lambda e: e.activation(out=r[:, 0:n], in_=ms[:, 0:n], func=AF.Ln, bias=EPS, scale=1.0),
                       [ms], [r])
                    op("act", lambda e: e.activation(out=r[:, 0:n], in_=r[:, 0:n], func=AF.Exp, scale=-0.5),
                       [r], [r])

                def rope(src, dst, h0, h1, off, npair, cos_ap, sin_ap, eng, rt):
                    nh = h1 - h0
                    sv = src[:, h0:h1, off:off + 2 * npair].rearrange("p h (i two) -> p h i two", two=2)
                    dv = dst[:, h0:h1, off:off + 2 * npair].rearrange("p h (i two) -> p h i two", two=2)
                    x1, x2 = sv[:, :, :, 0], sv[:, :, :, 1]
                    cb = cos_ap.unsqueeze(1).to_broadcast([128, nh, npair])
                    sbb = sin_ap.unsqueeze(1).to_broadcast([128, nh, npair])
                    t = [r_[:, 0:nh, 0:npair] for r_ in rt]
                    op(eng, lambda e: e.tensor_tensor(out=t[0], in0=x1, in1=cb, op=ALU.mult), [src, con], [rt[0]])
                    op(eng, lambda e: e.tensor_tensor(out=t[1], in0=x2, in1=sbb, op=ALU.mult), [src, con], [rt[1]])
                    op(eng, lambda e: e.tensor_tensor(out=t[2], in0=x1, in1=sbb, op=ALU.mult), [src, con], [rt[2]])
                    op(eng, lambda e: e.tensor_tensor(out=t[3], in0=x2, in1=cb, op=ALU.mult), [src, con], [rt[3]])
                    op(eng, lambda e: e.tensor_tensor(out=dv[:, :, :, 0], in0=t[0], in1=t[1], op=ALU.subtract),
                       [rt[0], rt[1]], [dst])
                    op(eng, lambda e: e.tensor_tensor(out=dv[:, :, :, 1], in0=t[2], in1=t[3], op=ALU.add),
                       [rt[2], rt[3]], [dst])

                B = banks

                def load_x(t):
                    X = xt[t % 2]
                    dma("sp", out=X[:], in_=x_d[t * 128:(t + 1) * 128, :], writes=[X], sb=X)

                def early(t):
                    par = t % 2
                    X = xt[par]
                    XT = xnT[par]
                    issue_casts(1)
                    if t + 1 < nt:
                        load_x(t + 1)
                    op("act", lambda e: e.activation(out=xn[:], in_=X[:], func=AF.Square, scale=float(D) ** -0.5,
                                                     accum_out=ms1[:, 0:1]), [X], [xn, ms1])
                    rstd(ms1, r1, 1)
                    op("dve", lambda e: e.scalar_tensor_tensor(out=xn[:], in0=X[:], scalar=r1[:, 0:1], in1=gar[:],
                                                               op0=ALU.mult, op1=ALU.mult), [X, r1, gar], [xn])
                    yield
                    for c in range(8):
                        op("pe", lambda e, c=c: e.transpose(out=bf(B[0])[:, c * 128:(c + 1) * 128],
                                                            in_=xn[:, c * 128:(c + 1) * 128], identity=ident_bf[:]),
                           [xn, ident_bf], [B[0]], inc=(c == 7))
                    op("act", lambda e: e.copy(out=XT[:].rearrange("p c s -> p (c s)"), in_=bf(B[0])), [B[0]], [XT])
                    dma("sp", out=xnt_d[t], in_=XT[:].rearrange("p c s -> p (c s)"), reads=[XT], writes=[xnt_b[t]],
                        sb=XT)
                    yield
                    blocks = [(0, 512), (512, 1024), (1024, 1536), (1536, 1824)]
                    for bi, (a, b) in enumerate(blocks):
                        for c in range(8):
                            op("pe", lambda e, bi=bi, a=a, b=b, c=c: e.matmul(
                                B[1 + bi][:, 0:b - a], lhsT=XT[:, c, :], rhs=wA[:, c, a:b], start=(c == 0),
                                stop=(c == 7)), [XT, wA], [B[1 + bi]], inc=(c == 7))
                        if bi == 1:
                            yield
                    B0, B1, B2, B3 = B[1], B[2], B[3], B[4]
                    yield
                    op("act", lambda e: e.activation(out=xn[:, 0:512], in_=B0[:], func=AF.Square,
                                                     scale=768.0 ** -0.5, accum_out=ms2[:, 0:1]), [B0], [xn, ms2])
                    op("act", lambda e: e.activation(out=xn[:, 512:768], in_=B1[:, 0:256], func=AF.Square,
                                                     scale=768.0 ** -0.5, accum_out=ms2[:, 1:2]), [B1], [xn, ms2])
                    op("act", lambda e: e.activation(out=xn[:, 768:1024], in_=B1[:, 256:512], func=AF.Square,
                                                     scale=256.0 ** -0.5, accum_out=ms2[:, 2:3]), [B1], [xn, ms2])
                    op("dve", lambda e: e.tensor_tensor(out=ms2[:, 0:1], in0=ms2[:, 0:1], in1=ms2[:, 1:2],
                                                        op=ALU.add), [ms2], [ms2])
                    rstd(ms2, r2, 3)
                    op("dve", lambda e: e.tensor_copy(out=EG[par][:, 0:512], in_=B2[:, :]), [B2], [EG[par]])
                    op("dve", lambda e: e.tensor_copy(out=EG[par][:, 512:800], in_=B3[:, 0:288]), [B3], [EG[par]])
                    yield
                    op("dve", lambda e: e.tensor_copy(out=cqk[:, 0:512], in_=B0[:]), [B0], [cqk])
                    op("dve", lambda e: e.tensor_copy(out=cqk[:, 512:1024], in_=B1[:, :]), [B1], [cqk])
                    yield
                    for c in range(8):
                        op("pe", lambda e, c=c: e.transpose(out=bf(B[0])[:, c * 128:(c + 1) * 128],
                                                            in_=cqk[:, c * 128:(c + 1) * 128], identity=ident_bf[:]),
                           [cqk, ident_bf], [B[0]], inc=(c == 7))
                    op("act", lambda e: e.copy(out=cT[:].rearrange("p c s -> p (c s)"), in_=bf(B[0])), [B[0]], [cT])
                    yield
                    PQ0, PQ1, PKV0, PKV1 = B[5], B[6], B[7], B[1]
                    for c in range(6):
                        op("pe", lambda e, c=c: e.matmul(PQ0[:, 0:480], lhsT=cT[:, c, :], rhs=wq[:, c, 0:480],
                                                         start=(c == 0), stop=(c == 5)), [cT, wq], [PQ0], inc=(c == 5))
                    for c in range(6):
                        op("pe", lambda e, c=c: e.matmul(PQ1[:, 0:288], lhsT=cT[:, c, :], rhs=wq[:, c, 480:768],
                                                         start=(c == 0), stop=(c == 5)), [cT, wq], [PQ1], inc=(c == 5))
                    for c in range(2):
                        op("pe", lambda e, c=c: e.matmul(PKV0[:], lhsT=cT[:, 6 + c, :], rhs=wkv[:, c, 0:512],
                                                         start=(c == 0), stop=(c == 1)), [cT, wkv], [PKV0], inc=(c == 1))
                    for c in range(2):
                        op("pe", lambda e, c=c: e.matmul(PKV1[:], lhsT=cT[:, 6 + c, :], rhs=wkv[:, c, 512:1024],
                                                         start=(c == 0), stop=(c == 1)), [cT, wkv], [PKV1], inc=(c == 1))
                    yield
                    eqf = EQ[par][:].rearrange("p h d -> p (h d)")
                    ekf = EKV[par][:].rearrange("p h d -> p (h d)")
                    op("act", lambda e: e.activation(out=eqf[:, 0:480], in_=PQ0[:, 0:480], func=AF.Copy,
                                                     scale=r2[:, 0:1]), [PQ0, r2], [EQ[par]])
                    op("dve", lambda e: e.tensor_scalar(out=eqf[:, 480:768], in0=PQ1[:, 0:288], scalar1=r2[:, 0:1],
                                                        scalar2=None, op0=ALU.mult), [PQ1, r2], [EQ[par]])
                    op("act", lambda e: e.activation(out=ekf[:, 0:512], in_=PKV0[:, :], func=AF.Copy,
                                                     scale=r2[:, 2:3]), [PKV0, r2], [EKV[par]])
                    op("dve", lambda e: e.tensor_scalar(out=ekf[:, 512:1024], in0=PKV1[:, :], scalar1=r2[:, 2:3],
                                                        scalar2=None, op0=ALU.mult), [PKV1, r2], [EKV[par]])
                    yield

                def late(t):
                    par = t % 2
                    j = t % 4
                    st_i = t // 4
                    Eq, Ekv, Eg = EQ[par], EKV[par], EG[par]
                    egq = Eg[:, 0:512].rearrange("p (h d) -> p h d", d=64)
                    egk = Eg[:, 512:640].rearrange("p (h d) -> p h d", d=64)
                    egv = Eg[:, 640:768].rearrange("p (h d) -> p h d", d=64)
                    ekr = Eg[:, 768:800]
                    op("act", lambda e: e.activation(out=qg[:], in_=Eq[:], func=AF.Square, scale=96.0 ** -0.5), [Eq], [qg])
                    op("act", lambda e: e.activation(out=kg[:, :, 0:64], in_=Ekv[:, :, 0:64], func=AF.Square,
                                                     scale=96.0 ** -0.5), [Ekv], [kg])
                    op("act", lambda e: e.activation(out=junk2[:, 0:32], in_=ekr, func=AF.Square,
                                                     scale=96.0 ** -0.5, accum_out=msr[:, 0:1]), [Eg], [junk2, msr])
                    op("act", lambda e: e.activation(out=gg[:].rearrange("p h d -> p (h d)"), in_=Eg[:, 0:640],
                                                     func=AF.Square, scale=64.0 ** -0.5), [Eg], [gg])
                    yield
                    op("dve", lambda e: e.tensor_reduce(out=msh[:, 0:8], in_=qg[:], axis=AX.X, op=ALU.add), [qg], [msh])
                    op("dve", lambda e: e.tensor_reduce(out=msh[:, 8:16], in_=kg[:, :, 0:64], axis=AX.X, op=ALU.add),
                       [kg], [msh])
                    op("dve", lambda e: e.tensor_scalar(out=msh[:, 8:16], in0=msh[:, 8:16], scalar1=msr[:, 0:1],
                                                        scalar2=None, op0=ALU.add), [msh, msr], [msh])
                    op("dve", lambda e: e.tensor_reduce(out=msh[:, 16:26], in_=gg[:], axis=AX.X, op=ALU.add), [gg], [msh])
                    rstd(msh, rh, 26)
                    yield
                    op("dve", lambda e: e.tensor_tensor(out=qn[:], in0=Eq[:],
                                                        in1=rh[:, 0:8].unsqueeze(2).to_broadcast([128, 8, 96]),
                                                        op=ALU.mult), [Eq, rh], [qn])
                    op("pool", lambda e: e.tensor_tensor(out=kn[:, :, 0:64], in0=Ekv[:, :, 0:64],
                                                         in1=rh[:, 8:16].unsqueeze(2).to_broadcast([128, 8, 64]),
                                                         op=ALU.mult), [Ekv, rh], [kn])
                    op("pool", lambda e: e.tensor_tensor(out=kn[:, :, 64:96],
                                                         in0=ekr.unsqueeze(1).to_broadcast([128, 8, 32]),
                                                         in1=rh[:, 8:16].unsqueeze(2).to_broadcast([128, 8, 32]),
                                                         op=ALU.mult), [Eg, rh], [kn])
                    op("dve", lambda e: e.tensor_tensor(out=gn[:, 0:8, :], in0=egq,
                                                        in1=rh[:, 16:24].unsqueeze(2).to_broadcast([128, 8, 64]),
                                                        op=ALU.mult), [Eg, rh], [gn])
                    op("dve", lambda e: e.tensor_tensor(out=gn[:, 8:10, :], in0=egk,
                                                        in1=rh[:, 24:26].unsqueeze(2).to_broadcast([128, 2, 64]),
                                                        op=ALU.mult), [Eg, rh], [gn])
                    op("act", lambda e: e.copy(out=vm[:, t, :, 0:64], in_=Ekv[:, :, 64:128]), [Ekv], [vm])
                    op("act", lambda e: e.copy(out=vg[:, t, :, 0:64], in_=egv), [Eg], [vg])
                    yield
                    op("pool", lambda e: e.tensor_tensor(out=qg[:], in0=qn[:],
                                                         in1=gmq_s[:].unsqueeze(1).to_broadcast([128, 8, 96]),
                                                         op=ALU.mult), [qn, gmq_s], [qg])
                    op("dve", lambda e: e.tensor_tensor(out=kg[:], in0=kn[:],
                                                        in1=vec[:, V_GMK:V_GMK + 96].unsqueeze(1).to_broadcast([128, 8, 96]),
                                                        op=ALU.mult), [kn, vec], [kg])
                    op("pool", lambda e: e.tensor_tensor(out=gg[:, 0:8, :], in0=gn[:, 0:8, :],
                                                         in1=ggq_s[:].unsqueeze(1).to_broadcast([128, 8, 64]),
                                                         op=ALU.mult), [gn, ggq_s], [gg])
                    op("dve", lambda e: e.tensor_tensor(out=gg[:, 8:10, :], in0=gn[:, 8:10, :],
                                                        in1=vec[:, V_GGK:V_GGK + 64].unsqueeze(1).to_broadcast([128, 2, 64]),
                                                        op=ALU.mult), [gn, vec], [gg])
                    yield
                    op("act", lambda e: e.copy(out=qf[:, :, 0:64], in_=qg[:, :, 0:64]), [qg], [qf])
                    op("act", lambda e: e.copy(out=kf[:, :, 0:64], in_=kg[:, :, 0:64]), [kg], [kf])
                    cm = con[:, C_COSM + t * 16:C_COSM + (t + 1) * 16]
                    sm = con[:, C_SINM + t * 16:C_SINM + (t + 1) * 16]
                    cgm = con[:, C_COSG + t * 32:C_COSG + (t + 1) * 32]
                    sgm = con[:, C_SING + t * 32:C_SING + (t + 1) * 32]
                    rope(kg, kf, 0, 8, 64, 16, cm, sm, "dve", rtk)
                    rope(qg, qf, 0, 8, 64, 16, cm, sm, "pool", rtq)
                    yield
                    rope(gg, gf, 0, 10, 0, 32, cgm, sgm, "pool", rtg)
                    yield
                    TQ, TK, TG = B[2], B[3], B[4]
                    for h in range(8):
                        op("pe", lambda e, h=h: e.transpose(out=bf(TK)[0:96, h * 128:(h + 1) * 128], in_=kf[:, h, :],
                                                            identity=ident_bf[:]), [kf, ident_bf], [TK], inc=(h == 7))
                    for h in range(8):
                        op("pe", lambda e, h=h: e.transpose(out=bf(TQ)[0:96, h * 128:(h + 1) * 128], in_=qf[:, h, :],
                                                            identity=ident_bf[:]), [qf, ident_bf], [TQ], inc=(h == 7))
                    op("dve", lambda e: e.tensor_copy(out=KTs[0:96, :, j * 128:(j + 1) * 128],
                                                      in_=bf(TK)[0:96, :].rearrange("p (h s) -> p h s", s=128)),
                       [TK], [KTs])
                    op("act", lambda e: e.copy(out=QTs[0:96, :, j * 128:(j + 1) * 128],
                                               in_=bf(TQ)[0:96, :].rearrange("p (h s) -> p h s", s=128)), [TQ], [QTs])
                    yield
                    for i in range(5):
                        op("pe", lambda e, i=i: e.transpose(
                            out=bf(TG)[:, i * 128:(i + 1) * 128],
                            in_=gf[:, 2 * i:2 * i + 2, :].rearrange("p h d -> p (h d)"),
                            identity=ident_bf[:]), [gf, ident_bf], [TG], inc=(i == 4))
                    op("act", lambda e: e.copy(out=QGs[:, :, j * 128:(j + 1) * 128],
                                               in_=bf(TG)[:, 0:512].rearrange("p (h s) -> p h s", s=128)), [TG], [QGs])
                    op("dve", lambda e: e.tensor_copy(out=KGs[:, j * 128:(j + 1) * 128], in_=bf(TG)[:, 512:640]),
                       [TG], [KGs])
                    if j == 3:
                        c0 = st_i * 512
                        dma("sp", out=qtm_d[:, :, c0:c0 + 512].rearrange("h d s -> d h s"), in_=QTs[0:96, :, :],
                            reads=[QTs], writes=[qtm_b[st_i]], sb=QTs)
                        dma("sp", out=ktm_d[:, :, c0:c0 + 512].rearrange("h d s -> d h s"), in_=KTs[0:96, :, :],
                            reads=[KTs], writes=[ktm_b[st_i]], sb=KTs)
                        dma("sp", out=qtg_d[:, :, c0:c0 + 512].rearrange("h d s -> d h s"), in_=QGs[:],
                            reads=[QGs], writes=[qtg_b[st_i]], sb=QGs)
                        dma("sp", out=ktg_d[:, c0:c0 + 512], in_=KGs[:], reads=[KGs], writes=[ktg_b[st_i]], sb=KGs)
                    yield

                load_x(0)
                for tick in range(nt + 1):
                    ge = early(tick) if tick < nt else iter(())
                    gl = late(tick - 1) if tick >= 1 else iter(())
                    alive = True
                    while alive:
                        a = next(ge, "end")
                        b = next(gl, "end")
                        alive = not (a == "end" and b == "end")
                kb.barrier()


        kb.ptr = P1
        yTa = kb.sb("yTa", [128, 4, S], BF16)
        yTb = kb.sb("yTb", [128, 4, S], BF16)
        P2 = kb.ptr
        p_sv0 = kb.ptr
        kb.ptr = P0
        wG = kb.sb("wG", [128, 8, 2048], BF16)
        pX = kb.ptr
        kb.ptr = p_sv0
        wG_loaded = [False]

        def load_wG():
            if not wG_loaded[0] and "D" in phases:
                wG_loaded[0] = True
                dma("pool", out=wG[:], in_=w_in_d[:, 1824:3872].rearrange("(c p) n -> p c n", p=128),
                    writes=[wG, vm, vg], sb=wG)

        if "B" in phases:
            Qb = [kb.sb("Qb%d" % i, [128, S], BF16) for i in range(2)]
            Kb = [kb.sb("Kb%d" % i, [128, S], BF16) for i in range(2)]
            NPT = 4
            pT = [kb.sb("pT%d" % i, [128, 1024], BF16) for i in range(NPT)]
            rl = kb.sb("rl", [128, 512], F32)
            rlb = [kb.sb("rlb%d" % i, [128, 512], F32) for i in range(2)]
            ytmp = [kb.sb("ytmp%d" % i, [128, 512], BF16) for i in range(2)]
            Vp = [kb.sb("Vp%d" % i, [128, NT, 128], BF16) for i in range(2)]
            for i in range(2):
                op("pool", lambda e, i=i: e.memset(Vp[i][:], 0.0), [], [Vp[i]])
            rl_d = nc.dram_tensor("rl_s", [4, 512], F32, kind="Internal").ap()
            rl_db = [Buf("rl_db%d" % i) for i in range(4)]
            NSP = 3
            SP_ = [Tile("spair%d" % i, pairs[i]) for i in range(NSP)]
            for t_ in SP_:
                t_.excl = True
            OB_ = banks[6:8]
            nqc = s_len // 512
            jobs = [("m", h) for h in range(8)] + [("g", h) for h in range(8)]
            qtm_all, ktm_all, qtg_all, ktg_all = qtm_b, ktm_b, qtg_b, ktg_b
            ktg_v = ktg_d.rearrange("(j d) s -> j d s", d=64)

            def load_job(i):
                kind, h = jobs[i]
                sl = i % 2
                issue_casts(1)
                Vsrc = vm if kind == "m" else vg
                hvv = h if kind == "m" else h // 4
                op("pool", lambda e: e.tensor_copy(out=Vp[sl][:, 0:nt, 0:65], in_=Vsrc[:, 0:nt, hvv, 0:65]),
                   [Vsrc], [Vp[sl]])
                if i == len(jobs) - 1:
                    load_wG()
                if kind == "m":
                    dma("sp", out=Qb[sl][0:96, 0:s_len], in_=qtm_d[h, :, 0:s_len], reads=qtm_all, writes=[Qb[sl]],
                        sb=Qb[sl])
                    dma("sp", out=Kb[sl][0:96, 0:s_len], in_=ktm_d[h, :, 0:s_len], reads=ktm_all, writes=[Kb[sl]],
                        sb=Kb[sl])
                else:
                    dma("sp", out=Qb[sl][:, 0:s_len], in_=qtg_d[h // 2, :, 0:s_len], reads=qtg_all, writes=[Qb[sl]],
                        sb=Qb[sl])
                    zh = 64 if h % 2 == 0 else 0
                    op("pool", lambda e: e.memset(Kb[sl][zh:zh + 64, 0:s_len], 0.0), [], [Kb[sl]])
                    dma("sp", out=Kb[sl][64 - zh:128 - zh, 0:s_len], in_=ktg_v[h // 4, :, 0:s_len], reads=ktg_all,
                        writes=[Kb[sl]], sb=Kb[sl])

            upairs = []
            for i, (kind, h) in enumerate(jobs):
                for qc in range(nqc):
                    for kp in range(nt // 2):
                        upairs.append((i, qc, kp))
            LAG = 2
            FIN_LAG = 4
            pending_fin = []
            load_job(0)
            yh_b = {}

            def emit_qk(p):
                i, qc, kp = upairs[p]
                kind, h = jobs[i]
                dk = 96 if kind == "m" else 128
                sl = i % 2
                Sp = SP_[p % NSP]
                for hh in range(2):
                    kt = 2 * kp + hh
                    op("pe", lambda e, kt=kt, hh=hh: e.matmul(
                        Sp[:, hh * 512:(hh + 1) * 512], lhsT=Kb[sl][0:dk, kt * 128:(kt + 1) * 128],
                        rhs=Qb[sl][0:dk, qc * 512:(qc + 1) * 512], start=True, stop=True),
                       [Kb[sl], Qb[sl]], [Sp], inc=(hh == 1))
                op("act", lambda e: e.activation(out=pT[p % NPT][:], in_=Sp[:, :], func=AF.Exp), [Sp], [pT[p % NPT]])

            def emit_pv(p, step):
                i, qc, kp = upairs[p]
                kind, h = jobs[i]
                V = Vp[i % 2]
                qi = i * nqc + qc
                O = OB_[qi % 2]
                for hh in range(2):
                    kt = 2 * kp + hh
                    op("pe", lambda e, kt=kt, hh=hh: e.matmul(
                        O[:, :], lhsT=V[:, kt, :], rhs=pT[p % NPT][:, hh * 512:(hh + 1) * 512],
                        start=(kt == 0), stop=(kt == nt - 1)), [V, pT[p % NPT]], [O], inc=(hh == 1))
                if kp == nt // 2 - 1:
                    yT = yTa if kind == "m" else yTb
                    key = (kind, h)
                    if key not in yh_b:
                        yh_b[key] = Buf("yh_%s%d" % key)
                    yb = yh_b[key]
                    rb = rlb[qi % 2]
                    op("dve", lambda e: e.reciprocal(out=rl[64:65, :], in_=O[64:65, :]), [O], [rl])
                    rsl = qi % 4
                    dma("pool", out=rl_d[rsl:rsl + 1, :], in_=rl[64:65, :], reads=[rl], writes=[rl_db[rsl]], sb=rl)
                    dma("pool", out=rb[0:64, :], in_=rl_d[rsl:rsl + 1, :].to_broadcast([64, 512]), reads=[rl_db[rsl]],
                        writes=[rb], sb=rb)

                    def fin():
                        if h % 2 == 0:
                            op("dve", lambda e: e.tensor_tensor(out=yT[0:64, h // 2, qc * 512:(qc + 1) * 512],
                                                                in0=O[0:64, :], in1=rb[0:64, :], op=ALU.mult),
                               [O, rb], [yb])
                        else:
                            yt = ytmp[(qi // 2) % 2]
                            op("dve", lambda e: e.tensor_tensor(out=yt[0:64, :], in0=O[0:64, :], in1=rb[0:64, :],
                                                                op=ALU.mult), [O, rb], [yt])
                            dma("pool", out=yT[64:128, h // 2, qc * 512:(qc + 1) * 512], in_=yt[0:64, :],
                                reads=[yt], writes=[yb], sb=yt)
                    pending_fin.append((step + FIN_LAG, fin))

            nU = len(upairs)
            for step in range(nU + LAG + FIN_LAG + 1):
                if step < nU:
                    emit_qk(step)
                if 0 <= step - LAG < nU:
                    emit_pv(step - LAG, step)
                if step < nU:
                    i_, qc_, kp_ = upairs[step]
                    if qc_ == 0 and kp_ == LAG and i_ + 1 < len(jobs):
                        load_job(i_ + 1)
                while pending_fin and pending_fin[0][0] <= step:
                    pending_fin.pop(0)[1]()
            issue_casts(100)
            kb.barrier()

        x1_b = [Buf("x1_b%d" % i) for i in range(nt)]
        h2_b = [Buf("h2_b%d" % i) for i in range(nt)]
        if "D" in phases:
            kb.ptr = pX
            h2fb = kb.sb("h2fb", [128, D], F32)
            h2T = kb.sb("h2T", [128, 8, 128], F32)
            assert kb.ptr <= P1
            kb.ptr = P2
            wmb = kb.sb("wmb", [128, 4, D], BF16)
            wgb = kb.sb("wgb", [128, 4, D], BF16)
            wout = kb.sb("wout", [128, 8, D], BF16)
            xnTc = kb.sb("xnTc", [128, 4, D], BF16)
            gsa = kb.sb("gsa", [128, 512], BF16)
            gsb = kb.sb("gsb", [128, 512], BF16)
            t1 = kb.sb("t1", [128, 512], F32)
            t2 = kb.sb("t2", [128, 512], F32)
            mT = kb.sb("mT", [128, 8, 512], BF16)
            xt2 = [kb.sb("xtD%d" % i, [128, D], F32) for i in range(2)]
            h2fa = kb.sb("h2f", [128, D], F32)
            h2fs = [h2fa, h2fb]
            h2bt = kb.sb("h2bt", [128, D], BF16)
            msD = kb.sb("msD", [128, 4], F32)
            rD = kb.sb("rD", [128, 4], F32)
            rDall = kb.sb("rDall", [128, NT], F32)
            load_wG()
            dma("pool", out=wmb[:], in_=w_mb_d.rearrange("(c p) n -> p c n", p=128), writes=[wmb], sb=wmb)
            dma("pool", out=wgb[:], in_=w_gb_d.rearrange("(c p) n -> p c n", p=128), writes=[wgb], sb=wgb)
            dma("pool", out=wout[:], in_=w_out_d.rearrange("(c p) n -> p c n", p=128), writes=[wout], sb=wout)
            GA, GB, ZA, ZB = banks[0], banks[1], banks[2], banks[3]
            X1 = banks[4:6]
            HT = banks[6:8]
            nsc = s_len // 512
            def load_xn(sc):
                dma("sp", out=xnTc[:], in_=xnt_d[4 * sc:4 * sc + 4].rearrange("t p n -> p t n"),
                    reads=xnt_b[4 * sc:4 * sc + 4], writes=[xnTc], sb=xnTc)

            def router_tr(t):
                h2f = h2fs[t % 2]
                for c in range(8):
                    op("pe", lambda e, c=c: e.transpose(out=HT[c // 4][:, (c % 4) * 128:(c % 4 + 1) * 128],
                                                        in_=h2f[:, c * 128:(c + 1) * 128],
                                                        identity=con[:, C_ID:C_ID + 128]),
                       [h2f, con], [HT[c // 4]], inc=(c % 4 == 3))
                for k2 in range(2):
                    op("act", lambda e, k2=k2: e.copy(out=h2T[:, 4 * k2:4 * k2 + 4, :].rearrange("p c s -> p (c s)"),
                                                      in_=HT[k2][:, :]), [HT[k2]], [h2T])

            def router_mm(t):
                for c in range(8):
                    op("pe", lambda e, c=c: e.matmul(RL[:, 0:NE], lhsT=h2T[:, c, :], rhs=wr[:, c, :],
                                                     start=(c == 0), stop=(c == 7)), [h2T, wr], [RL], inc=(c == 7))
                op("dve", lambda e: e.scalar_tensor_tensor(out=lg[:, t, :], in0=RL[:, 0:NE], scalar=rDall[:, t:t + 1],
                                                           in1=vec[:, V_BR:V_BR + NE], op0=ALU.mult, op1=ALU.add),
                   [RL, rDall, vec], [lg])

            RL = banks[0]
            pend_tr, pend_mm = [], []
            dma("sp", out=xt2[0][:], in_=x_d[0:128, :], writes=[xt2[0]], sb=xt2[0])

            def slot():
                if pend_mm:
                    router_mm(pend_mm.pop(0))
                if pend_tr:
                    t_ = pend_tr.pop(0)
                    router_tr(t_)
                    pend_mm.append(t_)
            load_xn(0)
            for sc in range(nsc):
                for dc in range(8):
                    if dc == 0:
                        slot()
                    for (G, off) in ((GA, 0), (GB, 1024)):
                        for c in range(8):
                            op("pe", lambda e, G=G, off=off, c=c: e.matmul(
                                G[:, :], lhsT=wG[:, c, off + dc * 128:off + (dc + 1) * 128],
                                rhs=xnTc[:, :, c * 128:(c + 1) * 128], start=(c == 0), stop=(c == 7)),
                               [wG, xnTc], [G], inc=(c == 7))
                    for (Z, wb, yT) in ((ZA, wmb, yTa), (ZB, wgb, yTb)):
                        for i in range(4):
                            op("pe", lambda e, Z=Z, wb=wb, yT=yT, i=i: e.matmul(
                                Z[:, :], lhsT=wb[:, i, dc * 128:(dc + 1) * 128],
                                rhs=yT[:, i, sc * 512:(sc + 1) * 512], start=(i == 0), stop=(i == 3)),
                               [wb, yT], [Z], inc=(i == 3))
                    op("act", lambda e: e.activation(out=gsa[:], in_=GA[:, :], func=AF.Sigmoid,
                                                     bias=vec[:, V_BG + dc:V_BG + dc + 1], scale=1.0), [GA, vec], [gsa])
                    op("act", lambda e: e.activation(out=gsb[:], in_=GB[:, :], func=AF.Sigmoid,
                                                     bias=vec[:, V_BG + 8 + dc:V_BG + 8 + dc + 1], scale=1.0),
                       [GB, vec], [gsb])
                    op("dve", lambda e: e.tensor_tensor(out=t1[:], in0=ZA[:, :], in1=gsa[:], op=ALU.mult), [ZA, gsa], [t1])
                    op("dve", lambda e: e.tensor_tensor(out=t2[:], in0=ZB[:, :], in1=gsb[:], op=ALU.mult), [ZB, gsb], [t2])
                    op("dve", lambda e: e.tensor_tensor(out=mT[:, dc, :], in0=t1[:], in1=t2[:], op=ALU.add),
                       [t1, t2], [mT])
                if sc + 1 < nsc:
                    load_xn(sc + 1)
                for j in range(4):
                    t = 4 * sc + j
                    X = xt2[t % 2]
                    h2f = h2fs[t % 2]
                    if t + 1 < nt:
                        Xn = xt2[(t + 1) % 2]
                        dma("sp", out=Xn[:], in_=x_d[(t + 1) * 128:(t + 2) * 128, :], writes=[Xn], sb=Xn)
                    for hf in range(2):
                        for dc in range(8):
                            op("pe", lambda e, hf=hf, dc=dc: e.matmul(
                                X1[hf][:, :], lhsT=mT[:, dc, j * 128:(j + 1) * 128],
                                rhs=wout[:, dc, hf * 512:(hf + 1) * 512], start=(dc == 0), stop=(dc == 7)),
                               [mT, wout], [X1[hf]], inc=(dc == 7))
                    for hf in range(2):
                        op("dve", lambda e, hf=hf: e.tensor_tensor(out=X[:, hf * 512:(hf + 1) * 512], in0=X1[hf][:, :],
                                                                   in1=X[:, hf * 512:(hf + 1) * 512], op=ALU.add),
                           [X1[hf], X], [X])
                    slot()
                    dma("sp", out=out_d[t * 128:(t + 1) * 128, :], in_=X[:], reads=[X], writes=[x1_b[t]], sb=X)
                    op("act", lambda e: e.activation(out=h2bt[:], in_=X[:], func=AF.Square, scale=float(D) ** -0.5,
                                                     accum_out=msD[:, 0:1]), [X], [h2bt, msD])
                    op("act", lambda e: e.activation(out=rD[:, 0:1], in_=msD[:, 0:1], func=AF.Ln, bias=EPS, scale=1.0),
                       [msD], [rD])
                    op("act", lambda e: e.activation(out=rDall[:, t:t + 1], in_=rD[:, 0:1], func=AF.Exp, scale=-0.5),
                       [rD], [rDall])
                    op("dve", lambda e: e.tensor_tensor(out=h2f[:], in0=X[:], in1=vec[:, V_GF:V_GF + D], op=ALU.mult),
                       [X, vec], [h2f])
                    op("dve", lambda e: e.tensor_scalar(out=h2bt[:], in0=h2f[:], scalar1=rDall[:, t:t + 1],
                                                        scalar2=None, op0=ALU.mult), [h2f, rDall], [h2bt])
                    dma("sp", out=h2_d[t * 128:(t + 1) * 128, :], in_=h2bt[:], reads=[h2bt], writes=[h2_b[t]], sb=h2bt)
                    pend_tr.append(t)
            while pend_tr or pend_mm:
                slot()
            kb.barrier()

        if "E" in phases:
            kb.ptr = P0E
            cap = nt * 16
            ncc = cap // 128
            NTE = nt * NE
            aff = kb.sb("aff", [128, nt, NE], F32)
            p_dead = kb.ptr
            ex = kb.sb("ex", [128, NT, NE], F32)
            base = kb.sb("base", [128, NT, NE], F32)
            pos = kb.sb("pos", [128, NT, NE], F32)
            gr1 = kb.sb("gr1", [128, NT, NE], F32)
            assert kb.ptr - p_dead == 8192
            p_dead2 = kb.ptr
            cmpb = kb.sb("cmpb", [128, NE, NT], BF16)
            ghi = kb.sb("ghi", [128, NT, NE], BF16)
            maskb = kb.sb("maskb", [128, NT, NE], BF16)
            padb = kb.sb("padb", [128, 512], BF16)
            assert kb.ptr - p_dead2 == 4096
            mx = kb.sb("mx", [128, nt], F32)
            se = kb.sb("se", [128, nt], F32)
            rse = kb.sb("rse", [128, nt], F32)
            part = kb.sb("part", [128, NE], F32)
            lo = kb.sb("lo", [128, NE], F32)
            mid = kb.sb("mid", [128, NE], F32)
            sst = kb.sb("sst", [128, NE], F32)
            posm = kb.sb("posm", [128, nt, NE], F32)
            vals = kb.sb("vals", [128, nt, NE, 6], BF16)
            RB = banks[0]
            op("dve", lambda e: e.tensor_reduce(out=mx[:], in_=lg[:, 0:nt, :], axis=AX.X, op=ALU.max), [lg], [mx])
            op("dve", lambda e: e.tensor_tensor(out=ex[:, 0:nt, :], in0=lg[:, 0:nt, :],
                                                in1=mx[:].unsqueeze(2).to_broadcast([128, nt, NE]), op=ALU.subtract),
               [lg, mx], [ex])
            op("act", lambda e: e.activation(out=ex[:, 0:nt, :], in_=ex[:, 0:nt, :], func=AF.Exp), [ex], [ex])
            op("dve", lambda e: e.tensor_reduce(out=se[:], in_=ex[:, 0:nt, :], axis=AX.X, op=ALU.add), [ex], [se])
            op("dve", lambda e: e.reciprocal(out=rse[:], in_=se[:]), [se], [rse])
            op("dve", lambda e: e.tensor_tensor(out=aff[:], in0=ex[:, 0:nt, :],
                                                in1=rse[:].unsqueeze(2).to_broadcast([128, nt, NE]), op=ALU.mult),
               [ex, rse], [aff])
            affT = ex[:, 0:nt, :].rearrange("p t e -> p (t e)").rearrange("p (e t) -> p e t", t=nt)
            op("dve", lambda e: e.tensor_copy(out=affT, in_=aff[:].rearrange("p t e -> p e t")), [aff], [ex])
            op("dve", lambda e: e.memset(lo[:], 0.0), [], [lo])
            op("dve", lambda e: e.memset(mid[:], 0.5), [], [mid])
            NIT = 30
            for k in range(NIT):
                half = 2.0 ** -(k + 1)
                op("dve", lambda e: e.tensor_tensor(out=cmpb[:, :, 0:nt], in0=affT,
                                                    in1=mid[:].unsqueeze(2).to_broadcast([128, NE, nt]), op=ALU.is_ge),
                   [ex, mid], [cmpb])
                op("dve", lambda e: e.tensor_reduce(out=part[:], in_=cmpb[:, :, 0:nt], axis=AX.X, op=ALU.add), [cmpb], [part])
                op("pe", lambda e: e.matmul(RB[:, 0:NE], lhsT=ones_f[:], rhs=part[:], start=True, stop=True),
                   [ones_f, part], [RB])
                op("dve", lambda e, half=half: e.tensor_scalar(out=sst[:], in0=RB[:, 0:NE], scalar1=cap - 0.5,
                                                               scalar2=half, op0=ALU.is_ge, op1=ALU.mult), [RB], [sst])
                op("dve", lambda e: e.tensor_tensor(out=lo[:], in0=lo[:], in1=sst[:], op=ALU.add), [lo, sst], [lo])
                op("dve", lambda e, half=half: e.tensor_scalar(out=mid[:], in0=lo[:], scalar1=half * 0.5, scalar2=None,
                                                               op0=ALU.add), [lo], [mid])
            op("dve", lambda e: e.tensor_tensor(out=maskb[:, 0:nt, :], in0=aff[:],
                                                in1=lo[:].unsqueeze(1).to_broadcast([128, nt, NE]), op=ALU.is_ge),
               [aff, lo], [maskb])
            WB, TB = banks[1], banks[2]
            mflat = maskb[:, 0:nt, :].rearrange("p t e -> p (t e)")
            op("pe", lambda e: e.matmul(WB[:, 0:NTE], lhsT=tri_bf[:], rhs=mflat, start=True, stop=True),
               [tri_bf, maskb], [WB])
            op("pe", lambda e: e.matmul(TB[:, 0:NTE], lhsT=ones_bf[:], rhs=mflat, start=True, stop=True),
               [ones_bf, maskb], [TB])
            op("dve", lambda e: e.memset(base[:, 0, :], 0.0), [], [base])
            for t in range(1, nt):
                op("dve", lambda e, t=t: e.tensor_tensor(out=base[:, t, :], in0=base[:, t - 1, :],
                                                         in1=TB[:, (t - 1) * NE:t * NE], op=ALU.add), [base, TB], [base])
            op("dve", lambda e: e.tensor_tensor(out=pos[:, 0:nt, :].rearrange("p t e -> p (t e)"), in0=WB[:, 0:NTE],
                                                in1=base[:, 0:nt, :].rearrange("p t e -> p (t e)"), op=ALU.add), [WB, base], [pos])
            op("dve", lambda e: e.scalar_tensor_tensor(out=posm[:], in0=pos[:, 0:nt, :], scalar=1.0, in1=maskb[:, 0:nt, :],
                                                       op0=ALU.add, op1=ALU.mult), [pos, maskb], [posm])
            op("dve", lambda e: e.tensor_scalar(out=posm[:], in0=posm[:], scalar1=-1.0, scalar2=None, op0=ALU.add),
               [posm], [posm])
            op("dve", lambda e: e.memset(vals[:], 0.0), [], [vals])
            op("dve", lambda e: e.tensor_copy(out=vals[:, :, :, 0],
                                              in_=con[:, C_T:C_T + nt].unsqueeze(2).to_broadcast([128, nt, NE])),
               [con], [vals])
            op("dve", lambda e: e.tensor_copy(out=vals[:, :, :, 1],
                                              in_=con[:, C_P:C_P + 1].unsqueeze(2).to_broadcast([128, nt, NE])),
               [con], [vals])
            op("dve", lambda e: e.tensor_copy(out=ghi[:, 0:nt, :], in_=aff[:]), [aff], [ghi])
            op("dve", lambda e: e.tensor_copy(out=vals[:, :, :, 2], in_=ghi[:, 0:nt, :]), [ghi], [vals])
            op("dve", lambda e: e.tensor_tensor(out=gr1[:, 0:nt, :], in0=aff[:], in1=ghi[:, 0:nt, :], op=ALU.subtract), [aff, ghi], [gr1])
            op("dve", lambda e: e.tensor_copy(out=ghi[:, 0:nt, :], in_=gr1[:, 0:nt, :]), [gr1], [ghi])
            op("dve", lambda e: e.tensor_copy(out=vals[:, :, :, 3], in_=ghi[:, 0:nt, :]), [ghi], [vals])
            op("dve", lambda e: e.tensor_tensor(out=gr1[:, 0:nt, :], in0=gr1[:, 0:nt, :], in1=ghi[:, 0:nt, :], op=ALU.subtract), [gr1, ghi], [gr1])
            op("dve", lambda e: e.tensor_copy(out=vals[:, :, :, 4], in_=gr1[:, 0:nt, :]), [gr1], [vals])

            Wg = [kb.sb("Wg%d" % i, [128, 8, FF], BF16) for i in range(2)]
            Wu = [kb.sb("Wu%d" % i, [128, 8, FF], BF16) for i in range(2)]
            Wd = [kb.sb("Wd%d" % i, [128, 8, D], BF16) for i in range(2)]
            oh = [kb.sb("oh%d" % i, [128, 512], BF16) for i in range(4)]
            selsb = [kb.sb("selsb%d" % i, [128, 4, 8], F32) for i in range(2)]
            idx = [kb.sb("idx%d" % i, [128, 4], I32) for i in range(2)]
            gate = [kb.sb("gate%d" % i, [128, 4], F32) for i in range(2)]
            xe = [kb.sb("xe%d" % i, [128, 4, D], BF16) for i in range(2)]
            xeT = kb.sb("xeT", [128, 8, cap], BF16)
            hT = kb.sb("hT", [128, 8, cap], BF16)
            sg = [kb.sb("sg%d" % i, [128, cap], F32) for i in range(2)]
            yo = [kb.sb("yo%d" % i, [128, D], F32) for i in range(2)]
            p_save = kb.ptr
            kb.ptr = p_dead
            yo += [kb.sb("yo%d" % i, [128, D], F32) for i in (2, 3)]
            kb.ptr = p_dead2
            oh += [kb.sb("oh%d" % i, [128, 512], BF16) for i in (4, 5, 6, 7)]
            kb.ptr = p_save
            out_grp = [Buf("out_grp%d" % i) for i in range(2)]
            SEL = banks[0]
            SELB = banks[7]
            TP = banks[1:3]
            AB_, UB_ = banks[3], banks[4]
            YB = banks[5:7]

            def load_w(e_):
                sl = e_ % 2
                for k, W_ in enumerate((Wg, Wu, Wd)):
                    dma("sp", out=W_[sl][:], in_=web_d[k, e_].rearrange("(c p) n -> p c n", p=128),
                        reads=[web_b[k][e_]], writes=[W_[sl]], sb=W_[sl])

            ohc = [0]
            selT = kb.sb("selT", [128, cap], F32)

            def select_dve(e_):
                for t in range(nt):
                    o = oh[t % 8]
                    op("dve", lambda e, o=o, t=t: e.tensor_scalar(
                        out=o[:, 0:cap], in0=con[:, C_IOTA:C_IOTA + cap], scalar1=posm[:, t, e_:e_ + 1], scalar2=None,
                        op0=ALU.is_equal), [con, posm], [o])
                    yield

            def select(e_):
                sl = e_ % 2
                for t in range(nt):
                    o = oh[t % 8]
                    op("pe", lambda e, o=o, t=t: e.matmul(SEL[0:6, 0:cap], lhsT=vals[:, t, e_, :], rhs=o[:, 0:cap],
                                                          start=(t == 0), stop=(t == nt - 1)), [o, vals], [SEL],
                       inc=True)
                    yield
                op("act", lambda e: e.copy(out=selT[0:6, :], in_=SEL[0:6, 0:cap]), [SEL], [selT])
                for cc in range(ncc):
                    op("pe", lambda e, cc=cc: e.transpose(out=SELB[:, cc * 8:cc * 8 + 6],
                                                          in_=selT[0:6, cc * 128:(cc + 1) * 128],
                                                          identity=con[0:6, C_ID:C_ID + 6]), [selT, con], [SELB],
                       inc=(cc == ncc - 1))
                op("act", lambda e: e.copy(out=selsb[sl][:, 0:ncc, 0:6],
                                           in_=SELB[:, 0:ncc * 8].rearrange("p (c k) -> p c k", k=8)[:, :, 0:6]),
                   [SELB], [selsb[sl]])
                op("dve", lambda e: e.scalar_tensor_tensor(out=idx[sl][:, 0:ncc], in0=selsb[sl][:, 0:ncc, 0], scalar=128.0,
                                                           in1=selsb[sl][:, 0:ncc, 1], op0=ALU.mult, op1=ALU.add),
                   [selsb[sl]], [idx[sl]])
                op("dve", lambda e: e.tensor_reduce(out=gate[sl][:, 0:ncc], in_=selsb[sl][:, 0:ncc, 2:5], axis=AX.X,
                                                    op=ALU.add), [selsb[sl]], [gate[sl]])
                for cc in range(ncc):
                    dma("pool", reads=h2_b[0:nt] + [idx[sl]], writes=[xe[sl]], sb=xe[sl],
                        fn=lambda e, cc=cc: e.indirect_dma_start(
                            out=xe[sl][:, cc, :], out_offset=None, in_=h2_d[0:s_len, :],
                            in_offset=bass.IndirectOffsetOnAxis(ap=idx[sl][:, cc:cc + 1], axis=0)))

            def compute(e_, sel=None, seld=None):
                sl = e_ % 2

                def adv(g, n):
                    if g is not None:
                        for _ in range(n):
                            next(g, None)
                for cc in range(ncc):
                    T_ = TP[cc % 2]
                    for c in range(8):
                        op("pe", lambda e, c=c: e.transpose(out=bf(T_)[:, c * 128:(c + 1) * 128],
                                                            in_=xe[sl][:, cc, c * 128:(c + 1) * 128],
                                                            identity=ident_bf[:]), [xe[sl], ident_bf], [T_], inc=(c == 7))
                    eng = "act" if cc % 2 == 0 else "dve"
                    if eng == "act":
                        op("act", lambda e: e.copy(out=xeT[:, :, cc * 128:(cc + 1) * 128],
                                                   in_=bf(T_).rearrange("p (c s) -> p c s", s=128)), [T_], [xeT])
                    else:
                        op("dve", lambda e: e.tensor_copy(out=xeT[:, :, cc * 128:(cc + 1) * 128],
                                                          in_=bf(T_).rearrange("p (c s) -> p c s", s=128)), [T_], [xeT])
                for fc in range(8):
                    for c in range(8):
                        op("pe", lambda e, c=c: e.matmul(AB_[:, 0:cap], lhsT=Wg[sl][:, c, fc * 128:(fc + 1) * 128],
                                                         rhs=xeT[:, c, :], start=(c == 0), stop=(c == 7)),
                           [Wg[sl], xeT], [AB_], inc=(c == 7))
                    for c in range(8):
                        op("pe", lambda e, c=c: e.matmul(UB_[:, 0:cap], lhsT=Wu[sl][:, c, fc * 128:(fc + 1) * 128],
                                                         rhs=xeT[:, c, :], start=(c == 0), stop=(c == 7)),
                           [Wu[sl], xeT], [UB_], inc=(c == 7))
                    s_ = sg[fc % 2]
                    adv(seld, (nt + 3) // 4)
                    op("act", lambda e: e.activation(out=s_[:], in_=AB_[:, 0:cap], func=AF.Silu), [AB_], [s_])
                    op("dve", lambda e: e.tensor_tensor(out=hT[:, fc, :], in0=UB_[:, 0:cap], in1=s_[:], op=ALU.mult),
                       [UB_, s_], [hT])
                    adv(sel, (nt + 3) // 4)
                    if fc == 3:
                        adv(seld, 1000)
                        adv(sel, 1000)
                adv(seld, 1000)
                adv(sel, 1000)
                out_grp[e_ % 2].w = {}
                out_grp[e_ % 2].r = {}
                for cc in range(ncc):
                    y_ = yo[cc % 4]
                    for hf in range(2):
                        Y = YB[hf]
                        for fc in range(8):
                            op("pe", lambda e, fc=fc: e.matmul(Y[:, :], lhsT=hT[:, fc, cc * 128:(cc + 1) * 128],
                                                               rhs=Wd[sl][:, fc, hf * 512:(hf + 1) * 512],
                                                               start=(fc == 0), stop=(fc == 7)),
                               [hT, Wd[sl]], [Y], inc=(fc == 7))
                        if hf == 0:
                            op("act", lambda e: e.activation(out=y_[:, 0:512], in_=Y[:, :], func=AF.Copy,
                                                             scale=gate[sl][:, cc:cc + 1]), [Y, gate[sl]], [y_])
                        else:
                            op("dve", lambda e: e.tensor_scalar(out=y_[:, 512:1024], in0=Y[:, :],
                                                                scalar1=gate[sl][:, cc:cc + 1], scalar2=None,
                                                                op0=ALU.mult), [Y, gate[sl]], [y_])
                    dma("pool", reads=[y_, idx[sl], out_grp[(e_ + 1) % 2]], merge=[out_grp[e_ % 2]], sb=y_,
                        fn=lambda e, cc=cc, y_=y_: e.indirect_dma_start(
                            out=out_d[0:s_len, :], out_offset=bass.IndirectOffsetOnAxis(ap=idx[sl][:, cc:cc + 1], axis=0),
                            in_=y_[:], in_offset=None, compute_op=ALU.add))

            n_exp = NE
            load_w(0)
            load_w(1)
            g0d, g0p = select_dve(0), select(0)
            for _ in range(nt):
                next(g0d, None)
                next(g0p, None)
            for _ in g0p:
                pass
            for e_ in range(n_exp):
                more = e_ + 1 < n_exp
                compute(e_, select(e_ + 1) if more else None, select_dve(e_ + 1) if more else None)
                if e_ + 2 < n_exp:
                    load_w(e_ + 2)
            kb.barrier()

        if debug and "D" not in phases:
            vm_o = nc.dram_tensor("vm_o", [128, NT * 8 * 66], BF16, kind="ExternalOutput").ap()
            vg_o = nc.dram_tensor("vg_o", [128, NT * 2 * 66], BF16, kind="ExternalOutput").ap()
            dma("sp", out=vm_o[:, 0:nt * 528], in_=vm[:, 0:nt].rearrange("p t h d -> p (t h d)"), reads=[vm], sb=vm)
            dma("sp", out=vg_o[:, 0:nt * 132], in_=vg[:, 0:nt].rearrange("p t h d -> p (t h d)"), reads=[vg], sb=vg)

        if debug and "B" in phases and "E" not in phases:
            ya_o = nc.dram_tensor("ya_o", [128, 4 * S], BF16, kind="ExternalOutput").ap()
            yb_o = nc.dram_tensor("yb_o", [128, 4 * S], BF16, kind="ExternalOutput").ap()
            dma("sp", out=ya_o.rearrange("p (i s) -> p i s", i=4)[:, :, 0:s_len], in_=yTa[:, :, 0:s_len], reads=[yTa], sb=yTa)
            dma("sp", out=yb_o.rearrange("p (i s) -> p i s", i=4)[:, :, 0:s_len], in_=yTb[:, :, 0:s_len], reads=[yTb], sb=yTb)
        if debug and "D" in phases:
            lg_o = nc.dram_tensor("lg_o", [128, NT * NE], F32, kind="ExternalOutput").ap()
            dma("sp", out=lg_o[:, 0:nt * NE], in_=lg[:, 0:nt, :].rearrange("p t e -> p (t e)"), reads=[lg], sb=lg)
        kb.finish()
        print("sbuf high water:", kb.hw, "semaphores used:", kb.nsem, {k: v.count for k, v in kb.E.items()})
    return nc


def make_in_maps(inputs):
    consts = make_consts()
    vecs = make_vecs(inputs)
    gar = np.ascontiguousarray(np.broadcast_to(np.asarray(inputs["g_attn_norm"], np.float32)[None, :], (128, D)))
    shared = {k: np.ascontiguousarray(np.asarray(inputs[k], np.float32)) for k in
              ("w_in", "w_q_up", "w_kv_up", "w_mla_branch", "w_gqa_branch", "w_out", "w_router",
               "w_exp_gate", "w_exp_up", "w_exp_down")}
    x = np.asarray(inputs["x"], np.float32)
    maps = []
    for b in range(x.shape[0]):
        m = dict(shared)
        m["x"] = np.ascontiguousarray(x[b])
        m["vecs"] = vecs
        m["gar"] = gar
        m["consts"] = consts
        maps.append(m)
    return maps


def kernel(**inputs):
    nc = build()
    maps = make_in_maps(inputs)
    res = run_bass_kernel_spmd(nc, maps, core_ids=list(range(8)))
    return np.stack([np.asarray(r["out"], np.float32) for r in res.results], axis=0)
```
